# Optimizing a Trainium2 kernel written in Bass

```python
import math
import jax, jax.numpy as jnp
from jax import lax
import numpy as np

D_MODEL = 1024
BATCH = 8
SEQ = 2048
DEPTH = 2

PLE_DIM = 256
NORM_EPS = 1e-6
N_BRANCHES = 3
BRANCH_WIDTH = D_MODEL // 2

M_HEADS = 4
M_DK = BRANCH_WIDTH // M_HEADS
M_DV = BRANCH_WIDTH // M_HEADS
M_CONV = 4
M_CHUNK = 128
M_FBIAS_LO = 3.0
M_FBIAS_HI = 6.0

A_HEADS = 4
A_DV = BRANCH_WIDTH // A_HEADS
A_DQK = A_DV // 2
Q_BLOCK = 128
ALIBI_SLOPES = tuple(2.0 ** (-8.0 * (h + 1) / A_HEADS) for h in range(A_HEADS))

P_WINDOWS = (2, 4, 8, 16)
P_GROUPS = len(P_WINDOWS)
P_GC = BRANCH_WIDTH // P_GROUPS

D_FF = 2816
N_EXPERTS = 8
TOP_K = 2
D_FF_EXPERT = 3584
N_DENSE = (DEPTH + 1) // 2
N_MOE = DEPTH // 2

IN_SPLITS = (2 * M_HEADS * M_DK,
             M_HEADS * M_DV,
             BRANCH_WIDTH,
             M_HEADS,
             M_HEADS,
             A_HEADS * 2 * A_DQK,
             A_HEADS * 2 * A_DQK,
             A_HEADS * A_DV,
             BRANCH_WIDTH,
             N_BRANCHES * D_MODEL)
IN_WIDTH = sum(IN_SPLITS)

kernel_name = "hybrid_mlstm_diffattn_pool_moe_block"

F32 = jnp.float32


def rms_norm(x, gain):
    xf = x.astype(F32)
    y = xf * lax.rsqrt(jnp.mean(xf * xf, axis=-1, keepdims=True) + NORM_EPS)
    return (y * gain.astype(F32)).astype(x.dtype)


def causal_dwconv(x, w, b):
    C = x.shape[-1]
    y = lax.conv_general_dilated(x, w[:, None, :].astype(x.dtype), window_strides=(1,),
                                 padding=[(w.shape[0] - 1, 0)],
                                 dimension_numbers=('NWC', 'WIO', 'NWC'),
                                 feature_group_count=C)
    return y + b.astype(x.dtype)


def swiglu(h, w_gu, w_down):
    g, u = jnp.split(h @ w_gu, 2, axis=-1)
    return (jax.nn.silu(g) * u) @ w_down


def mlstm_chunkwise(q, k, v, i_pre, f_pre):
    B, H, S, DK = q.shape
    DV = v.shape[-1]
    L = M_CHUNK
    NC = S // L
    logf = jax.nn.log_sigmoid(f_pre)

    def to_chunks(a):
        return jnp.moveaxis(a.reshape(B, H, NC, L, *a.shape[3:]), 2, 0)

    xs = (to_chunks(q), to_chunks(k), to_chunks(v), to_chunks(i_pre), to_chunks(logf))
    causal = jnp.tril(jnp.ones((L, L), dtype=bool))

    def step(carry, xc):
        C, n, m = carry
        qx, kx, vx, ix, fx = xc
        b = jnp.cumsum(fx, axis=-1)
        logD = jnp.where(causal, b[..., :, None] - b[..., None, :] + ix[..., None, :], -jnp.inf)
        inter = b + m[..., None]
        m_t = jnp.maximum(jnp.max(logD, axis=-1), inter)
        Dw = jnp.exp(logD - m_t[..., None])
        w_inter = jnp.exp(inter - m_t)
        s = jnp.einsum('bhtd,bhsd->bhts', qx, kx) * Dw
        num = jnp.einsum('bhts,bhsv->bhtv', s, vx) + w_inter[..., None] * jnp.einsum('bhtd,bhdv->bhtv', qx, C)
        den = jnp.sum(s, axis=-1) + w_inter * jnp.einsum('bhtd,bhd->bht', qx, n)
        h = num / jnp.maximum(jnp.abs(den), jnp.exp(-m_t))[..., None]
        bL = b[..., -1]
        log_w = bL[..., None] - b + ix
        m_new = jnp.maximum(bL + m, jnp.max(log_w, axis=-1))
        decay = jnp.exp(bL + m - m_new)
        w = jnp.exp(log_w - m_new[..., None])
        C_new = decay[..., None, None] * C + jnp.einsum('bhs,bhsd,bhsv->bhdv', w, kx, vx)
        n_new = decay[..., None] * n + jnp.einsum('bhs,bhsd->bhd', w, kx)
        return (C_new, n_new, m_new), h

    init = (jnp.zeros((B, H, DK, DV), F32), jnp.zeros((B, H, DK), F32), jnp.zeros((B, H), F32))
    _, hs = lax.scan(step, init, xs)
    return jnp.moveaxis(hs, 0, 2).reshape(B, H, S, DV)


def mlstm_branch(qk_pre, v_pre, o_pre, i_pre, f_pre, conv_w, conv_b, gate_bias, head_gain):
    B, S, _ = qk_pre.shape
    qk = jax.nn.silu(causal_dwconv(qk_pre, conv_w, conv_b))
    q, k = jnp.split(qk, 2, axis=-1)

    def heads(a, d):
        return a.reshape(B, S, M_HEADS, d).transpose(0, 2, 1, 3).astype(F32)

    q = heads(q, M_DK)
    k = heads(k, M_DK) * (M_DK ** -0.5)
    v = heads(v_pre, M_DV)
    gb = gate_bias.astype(F32)
    ig = (i_pre.astype(F32) + gb[:M_HEADS]).transpose(0, 2, 1)
    fg = (f_pre.astype(F32) + gb[M_HEADS:]).transpose(0, 2, 1)
    h = mlstm_chunkwise(q, k, v, ig, fg).transpose(0, 2, 1, 3)
    h = rms_norm(h, head_gain.reshape(M_HEADS, M_DV)).reshape(B, S, BRANCH_WIDTH)
    h = h * jax.nn.sigmoid(o_pre.astype(F32))
    return h.astype(qk_pre.dtype)


def diff_attention_branch(q_pre, k_pre, v_pre, q_gain, k_gain, lam_params, head_gain, lam_init):
    B, S, _ = q_pre.shape
    dt = q_pre.dtype
    q = rms_norm(q_pre.reshape(B, S, A_HEADS, 2, A_DQK), q_gain).astype(F32) * (A_DQK ** -0.5)
    k = rms_norm(k_pre.reshape(B, S, A_HEADS, 2, A_DQK), k_gain).astype(F32)
    q = q.transpose(0, 2, 3, 1, 4)
    k = k.transpose(0, 2, 3, 1, 4)
    v = v_pre.reshape(B, S, A_HEADS, A_DV).transpose(0, 2, 1, 3).astype(F32)
    lp = lam_params.astype(F32)
    lam = jnp.exp(jnp.dot(lp[0], lp[1])) - jnp.exp(jnp.dot(lp[2], lp[3])) + lam_init
    slopes = jnp.asarray(ALIBI_SLOPES, F32)[:, None, None, None]
    outs = []
    for blk in range(S // Q_BLOCK):
        q0 = blk * Q_BLOCK
        q1 = q0 + Q_BLOCK
        s = jnp.einsum('bhmqd,bhmkd->bhmqk', q[:, :, :, q0:q1], k[:, :, :, :q1])
        dist = (jnp.arange(q0, q1)[:, None] - jnp.arange(q1)[None, :]).astype(F32)
        s = jnp.where(dist >= 0, s - slopes * dist, -jnp.inf)
        a = jax.nn.softmax(s, axis=-1)
        a = a[:, :, 0] - lam * a[:, :, 1]
        outs.append(jnp.einsum('bhqk,bhkd->bhqd', a, v[:, :, :q1]))
    o = jnp.concatenate(outs, axis=2).transpose(0, 2, 1, 3)
    o = rms_norm(o, head_gain.reshape(A_HEADS, A_DV)) * (1.0 - lam_init)
    return o.reshape(B, S, BRANCH_WIDTH).astype(dt)


def pool_branch(u, w_group, scale):
    B, S, _ = u.shape
    uf = u.astype(F32).reshape(B, S, P_GROUPS, P_GC)
    cs = jnp.pad(jnp.cumsum(uf, axis=1), ((0, 0), (1, 0), (0, 0), (0, 0)))
    t = jnp.arange(S)
    outs = []
    for g, w in enumerate(P_WINDOWS):
        lower = jnp.pad(cs[:, :S - w + 1, g], ((0, 0), (w - 1, 0), (0, 0)))
        cnt = jnp.minimum(t + 1, w).astype(F32)[None, :, None]
        outs.append((cs[:, 1:, g] - lower) / cnt)
    pooled = jnp.stack(outs, axis=2) - uf
    y = jnp.einsum('bsgc,gcd->bsgd', pooled, w_group.astype(F32)).reshape(B, S, BRANCH_WIDTH)
    return (y * scale.astype(F32)).astype(u.dtype)


def moe_swiglu(h, w_router, b_router, w_gu, w_down):
    logits = h.astype(F32) @ w_router.astype(F32) + b_router.astype(F32)
    top_val, top_idx = lax.top_k(logits, TOP_K)
    top_gate = jax.nn.softmax(top_val, axis=-1)
    combine = jnp.sum(jax.nn.one_hot(top_idx, N_EXPERTS, dtype=F32) * top_gate[..., None], axis=-2)
    out = jnp.zeros_like(h)
    for e in range(N_EXPERTS):
        out = out + combine[..., e:e + 1].astype(h.dtype) * swiglu(h, w_gu[e], w_down[e])
    return out


def setup_inputs(seed: int = 0) -> dict:
    key = jax.random.key(seed)
    ks = jax.random.split(key, 28)
    D = D_MODEL

    def nrm(i, shape, scale):
        return scale * jax.random.normal(ks[i], shape, F32)

    def gain(i, shape):
        return 1.0 + nrm(i, shape, 0.05)

    f_bias = jnp.linspace(M_FBIAS_LO, M_FBIAS_HI, M_HEADS, dtype=F32)
    m_gate_bias = jnp.concatenate([nrm(6, (DEPTH, M_HEADS), 0.1),
                                   f_bias[None, :] + nrm(7, (DEPTH, M_HEADS), 0.1)], axis=-1)
    return {
        "x": nrm(0, (BATCH, SEQ, D), 1.0),
        "p": nrm(1, (DEPTH, BATCH, SEQ, PLE_DIM), 1.0),
        "attn_norm": gain(2, (DEPTH, D)),
        "w_in": nrm(3, (DEPTH, D, IN_WIDTH), D ** -0.5),
        "m_conv_w": nrm(4, (DEPTH, M_CONV, 2 * M_HEADS * M_DK), M_CONV ** -0.5),
        "m_conv_b": nrm(5, (DEPTH, 2 * M_HEADS * M_DK), 0.02),
        "m_gate_bias": m_gate_bias,
        "m_head_norm": gain(8, (DEPTH, BRANCH_WIDTH)),
        "a_q_norm": gain(9, (DEPTH, A_DQK)),
        "a_k_norm": gain(10, (DEPTH, A_DQK)),
        "a_lambda": nrm(11, (DEPTH, 4, A_DQK), 0.1),
        "a_head_norm": gain(12, (DEPTH, BRANCH_WIDTH)),
        "pool_w": nrm(13, (DEPTH, P_GROUPS, P_GC, P_GC), P_GC ** -0.5),
        "pool_scale": 1.0 + nrm(14, (DEPTH, BRANCH_WIDTH), 0.1),
        "w_branch": nrm(15, (DEPTH, N_BRANCHES, BRANCH_WIDTH, D), BRANCH_WIDTH ** -0.5),
        "w_out": nrm(16, (DEPTH, D, D), D ** -0.5),
        "ffn_norm": gain(17, (DEPTH, D)),
        "dense_w_gu": nrm(18, (N_DENSE, D, 2 * D_FF), D ** -0.5),
        "dense_w_down": nrm(19, (N_DENSE, D_FF, D), D_FF ** -0.5),
        "router_w": nrm(20, (N_MOE, D, N_EXPERTS), D ** -0.5),
        "router_b": nrm(21, (N_MOE, N_EXPERTS), 0.01),
        "moe_w_gu": nrm(22, (N_MOE, N_EXPERTS, D, 2 * D_FF_EXPERT), D ** -0.5),
        "moe_w_down": nrm(23, (N_MOE, N_EXPERTS, D_FF_EXPERT, D), D_FF_EXPERT ** -0.5),
        "ple_norm": gain(24, (DEPTH, D)),
        "ple_w_gate": nrm(25, (DEPTH, D, D), D ** -0.5),
        "ple_w_proj": nrm(26, (DEPTH, PLE_DIM, D), PLE_DIM ** -0.5),
    }


def reference(x, p, attn_norm, w_in, m_conv_w, m_conv_b, m_gate_bias, m_head_norm,
              a_q_norm, a_k_norm, a_lambda, a_head_norm, pool_w, pool_scale,
              w_branch, w_out, ffn_norm, dense_w_gu, dense_w_down, router_w, router_b,
              moe_w_gu, moe_w_down, ple_norm, ple_w_gate, ple_w_proj):
    B, S, D = x.shape
    split_points = np.cumsum(np.array(IN_SPLITS))[:-1].tolist()
    for layer in range(DEPTH):
        h = rms_norm(x, attn_norm[layer])
        (m_qk, m_v, m_o, m_i, m_f, a_q, a_k, a_v, p_u, g_pre) = jnp.split(h @ w_in[layer], split_points, axis=-1)
        h_m = mlstm_branch(m_qk, m_v, m_o, m_i, m_f, m_conv_w[layer], m_conv_b[layer],
                           m_gate_bias[layer], m_head_norm[layer])
        lam_init = 0.8 - 0.6 * math.exp(-0.3 * layer)
        h_a = diff_attention_branch(a_q, a_k, a_v, a_q_norm[layer], a_k_norm[layer],
                                    a_lambda[layer], a_head_norm[layer], lam_init)
        h_p = pool_branch(p_u, pool_w[layer], pool_scale[layer])
        hb = jnp.stack([h_m, h_a, h_p], axis=2)
        yb = jnp.einsum('bsrc,rcd->bsrd', hb, w_branch[layer])
        gates = jax.nn.sigmoid(g_pre.reshape(B, S, N_BRANCHES, D))
        x = x + jnp.sum(gates * yb, axis=2) @ w_out[layer]
        hf = rms_norm(x, ffn_norm[layer])
        if layer % 2 == 0:
            j = layer // 2
            x = x + swiglu(hf, dense_w_gu[j], dense_w_down[j])
        else:
            j = layer // 2
            x = x + moe_swiglu(hf, router_w[j], router_b[j], moe_w_gu[j], moe_w_down[j])
        pg = jax.nn.sigmoid(rms_norm(x, ple_norm[layer]) @ ple_w_gate[layer])
        x = x + pg * (p[layer] @ ple_w_proj[layer])
    return x
```

```python
import math
import numpy as np
import concourse.bass as bass
import concourse.mybir as mybir
from concourse.bass_utils import run_bass_kernel_spmd

F32 = mybir.dt.float32
BF16 = mybir.dt.bfloat16
AF = mybir.ActivationFunctionType
ALU = mybir.AluOpType
AX = mybir.AxisListType

ENGS = ("pe", "act", "dve", "pool", "sp")

D = 1024
T = 2048
DEPTH = 2
PLE = 256
EPS = 1e-6
KC = 8
TB = 512
NTB = 4
NTT = 16
IN_W = 7176
C_MQK, C_MV, C_MO, C_MI, C_MF = 0, 1024, 1536, 2048, 2052
C_AQ, C_AK, C_AV, C_PU, C_G = 2056, 2568, 3080, 3592, 4104
DFF = 2816
NE = 8
DFE = 3584
SLOPES = [2.0 ** (-8.0 * (h + 1) / 4) for h in range(4)]
PWIN = (2, 4, 8, 16)
QB = 256
LNS = math.log(128 ** -0.5)

CI, CU, CO, CB, CAL, CPC, NCONST = 0, 128, 256, 384, 512, 576, 640


def make_consts():
    c = np.zeros((128, NCONST), np.float32)
    p = np.arange(128)
    c[:, CI:CI + 128] = np.eye(128)
    c[:, CU:CU + 128] = (p[:, None] <= p[None, :])
    c[:, CO:CO + 128] = 1.0
    c[:, CB:CB + 128] = (p[:, None] // 64 == p[None, :] // 64)
    for h in range(4):
        for di in range(16):
            c[:, CAL + h * 16 + di] = SLOPES[h] * (p + 128.0 * (1 - di) - (QB - 1))
    for g, w in enumerate(PWIN):
        for t in range(16):
            c[:, CPC + g * 16 + t] = w / min(t + 1, w)
    return c


class Buf:
    __slots__ = ("name", "writer", "readers", "wsem", "rsem")

    def __init__(self, name=""):
        self.name = name
        self.writer = None
        self.readers = {}
        self.wsem = None
        self.rsem = None


class Sched:
    def __init__(self, nc, same_engine_sync=True):
        self.nc = nc
        self.ops = {e: [] for e in ENGS}
        self.cnt = {e: 0 for e in ENGS}
        self.seen = {e: {} for e in ENGS}
        self.sems = {}
        self.same_engine_sync = same_engine_sync
        import os as _os
        self.nosync_engs = set(_os.environ.get("NOSYNC", "pe").split(","))
        self.semcnt = {}
        self._stack = []
        for e in ENGS:
            self.sems[e] = self._alloc("s_" + e)
        self.nsem = len(ENGS)

    def _alloc(self, name):
        cm = self.nc.semaphore(name)
        h = cm.__enter__()
        self._stack.append(cm)
        return h

    def new_sem(self, name):
        key = "d%d_%s" % (self.nsem, name)
        self.nsem += 1
        self.sems[key] = self._alloc(key)
        return key

    def _waits(self, eng, reads, writes):
        need = {}

        def add(ev):
            if ev is None:
                return
            k, v = ev
            if k == eng and (not self.same_engine_sync or eng in self.nosync_engs):
                return
            if k in self.semcnt:
                v = self.semcnt[k]
            if need.get(k, 0) < v:
                need[k] = v

        for b in reads:
            add(b.writer)
        for b in writes:
            add(b.writer)
            for k, v in b.readers.items():
                add((k, v))
        out = []
        seen = self.seen[eng]
        for k, v in need.items():
            if seen.get(k, 0) < v:
                seen[k] = v
                out.append((k, v))
        return out

    def _mark(self, ev, reads, writes):
        k, v = ev
        for b in reads:
            if b.readers.get(k, 0) < v:
                b.readers[k] = v
        for b in writes:
            b.writer = ev
            b.readers = {}

    def op(self, eng, fn, reads=(), writes=()):
        waits = self._waits(eng, reads, writes)
        self.cnt[eng] += 1
        ev = (eng, self.cnt[eng])
        self._mark(ev, reads, writes)
        self.ops[eng].append((waits, fn, (eng, 1)))
        return ev

    def group(self, eng, fns, reads=(), writes=()):
        waits = self._waits(eng, reads, writes)
        self.cnt[eng] += 1
        ev = (eng, self.cnt[eng])
        self._mark(ev, reads, writes)
        n = len(fns)
        for i, fn in enumerate(fns):
            self.ops[eng].append((waits if i == 0 else [], fn, (eng, 1) if i == n - 1 else None))
        return ev

    def dma(self, queue, out_ap, in_ap, reads=(), writes=(), nowait=False, **kw):
        waits = [] if nowait else self._waits(queue, reads, writes)
        if writes:
            own = writes[0]
            if own.wsem is None:
                own.wsem = self.new_sem("w")
            key = own.wsem
        else:
            own = reads[0]
            if own.rsem is None:
                own.rsem = self.new_sem("r")
            key = own.rsem
        self.semcnt[key] = self.semcnt.get(key, 0) + 16
        ev = (key, self.semcnt[key])
        for b in writes:
            b.writer = ev
            b.readers = {}
        for b in reads:
            b.readers[key] = ev[1]

        def fn(e, out_ap=out_ap, in_ap=in_ap, kw=kw):
            return e.dma_start(out=out_ap, in_=in_ap, **kw)

        self.ops[queue].append((waits, fn, (key, 16)))
        return ev

    def wait_all(self, eng, bufs):
        need = {}
        for b in bufs:
            for ev in [b.writer] + list(b.readers.items()):
                if ev is None:
                    continue
                k, v = ev
                if need.get(k, 0) < v:
                    need[k] = v
        waits = []
        for k, v in need.items():
            if self.seen[eng].get(k, 0) < v:
                self.seen[eng][k] = v
                waits.append((k, v))
        self.ops[eng].append((waits, None, None))

    def barrier(self):
        tgt = {e: self.cnt[e] for e in ENGS if self.cnt[e] > 0}
        for k, v in self.semcnt.items():
            tgt[k] = v
        for e in ENGS:
            waits = []
            for k, v in tgt.items():
                if k == e:
                    continue
                if self.seen[e].get(k, 0) < v:
                    self.seen[e][k] = v
                    waits.append((k, v))
            if self.cnt[e] > 0 and self.seen[e].get(e, 0) < self.cnt[e]:
                self.seen[e][e] = self.cnt[e]
                waits.append((e, self.cnt[e]))
            self.ops[e].append((waits, None, None))

    def _emit(self, e, name):
        for waits, fn, inc in self.ops[name]:
            for k, v in waits:
                e.wait_ge(self.sems[k], v)
            if fn is None:
                continue
            ins = fn(e)
            if inc is None:
                continue
            ins.then_inc(self.sems[inc[0]], inc[1])

    def replay(self):
        with self.nc.Block() as block:
            @block.tensor
            def _(e):
                self._emit(e, "pe")

            @block.scalar
            def _(e):
                self._emit(e, "act")

            @block.vector
            def _(e):
                self._emit(e, "dve")

            @block.gpsimd
            def _(e):
                self._emit(e, "pool")

            @block.sync
            def _(e):
                self._emit(e, "sp")

    def close(self):
        while self._stack:
            self._stack.pop().__exit__(None, None, None)


class Prog:
    def __init__(self, stop_after=None, taps=()):
        self.stop_after = stop_after
        self.taps = list(taps)
        self.nc = bass.Bass("TRN2", target_bir_lowering=False)
        nc = self.nc
        self.S = Sched(nc)
        self._cms = []
        d = {}

        def din(name, shape):
            d[name] = nc.dram_tensor(name, list(shape), F32, kind="ExternalInput").ap()

        din("x", [T, D]); din("p", [DEPTH, T, PLE]); din("consts", [128, NCONST])
        din("attn_norm", [DEPTH, D]); din("w_in", [DEPTH, D, IN_W]); din("m_conv_w", [DEPTH, 4, D])
        din("m_conv_b", [DEPTH, D]); din("m_gate_bias", [DEPTH, 8]); din("m_head_norm", [DEPTH, 512])
        din("a_q_norm", [DEPTH, 64]); din("a_k_norm", [DEPTH, 64]); din("a_lambda", [DEPTH, 4, 64])
        din("a_head_norm", [DEPTH, 512]); din("pool_w", [DEPTH, 4, 128, 128]); din("pool_scale", [DEPTH, 512])
        din("w_branch", [DEPTH, 3, 512, D]); din("w_out", [DEPTH, D, D]); din("ffn_norm", [DEPTH, D])
        din("dense_w_gu", [1, D, 2 * DFF]); din("dense_w_down", [1, DFF, D]); din("router_w", [1, D, NE])
        din("router_b", [1, NE]); din("moe_w_gu", [1, NE, D, 2 * DFE]); din("moe_w_down", [1, NE, DFE, D])
        din("ple_norm", [DEPTH, D]); din("ple_w_gate", [DEPTH, D, D]); din("ple_w_proj", [DEPTH, PLE, D])
        self.d = d
        self.out = nc.dram_tensor("out", [T, D], F32, kind="ExternalOutput").ap()
        self.tap_out = {}

    def sb(self, name, shape, dt):
        cm = self.nc.sbuf_tensor(name, list(shape), dt)
        t = cm.__enter__()
        self._cms.append(cm)
        return t

    def psum(self, name):
        cm = self.nc.psum_tensor(name, [128, 512], F32)
        t = cm.__enter__()
        self._cms.append(cm)
        return t

    def act(self, out, in_, func, reads, writes, bias=0.0, scale=1.0):
        self.S.op("act", lambda e: e.activation(out=out, in_=in_, func=func, bias=bias, scale=scale), reads, writes)

    def tt(self, eng, out, in0, in1, op, reads, writes):
        self.S.op(eng, lambda e: e.tensor_tensor(out, in0, in1, op), reads, writes)

    def ts(self, eng, out, in0, s1, s2, op0, op1, reads, writes):
        if s2 is None:
            self.S.op(eng, lambda e: e.tensor_scalar(out, in0, s1, None, op0), reads, writes)
        else:
            self.S.op(eng, lambda e: e.tensor_scalar(out, in0, s1, s2, op0, op1), reads, writes)

    def stt(self, eng, out, in0, scalar, in1, op0, op1, reads, writes):
        self.S.op(eng, lambda e: e.scalar_tensor_tensor(out, in0, scalar, in1, op0, op1), reads, writes)

    def copy(self, eng, out, in_, reads, writes):
        if eng == "act":
            self.S.op("act", lambda e: e.activation(out=out, in_=in_, func=AF.Copy), reads, writes)
        else:
            self.S.op(eng, lambda e: e.tensor_copy(out, in_), reads, writes)

    def mm(self, out, pairs, reads, writes, start=True, stop=True):
        n = len(pairs)
        fns = []
        for i, (l, r) in enumerate(pairs):
            fns.append(lambda e, l=l, r=r, i=i: e.matmul(out, l, r, start=(start and i == 0), stop=(stop and i == n - 1)))
        self.S.group("pe", fns, reads, writes)

    def mm1(self, out, l, r, reads, writes, start=True, stop=True):
        self.S.op("pe", lambda e: e.matmul(out, l, r, start=start, stop=stop, skip_group_check=True), reads, writes)

    def transpose(self, out, in_, ident, reads, writes):
        self.S.op("pe", lambda e: e.transpose(out, in_, ident), reads, writes)

    def tap(self, name, ap, shape, dt, bufs):
        if name not in self.taps:
            return
        o = self.nc.dram_tensor("tap_" + name, list(shape), dt, kind="ExternalOutput").ap()
        self.tap_out[name] = o
        b = Buf("tap")
        self.S.dma("sp", o, ap, reads=list(bufs))
        self.tapbufs = getattr(self, "tapbufs", []) + list(bufs)

    def wload(self, dram_ap, view):
        i = self.wi % len(self.wbufs)
        self.wi += 1
        t, b = self.wbufs[i]
        dst = view(t)
        self.S.dma("pool", dst, dram_ap, writes=[b])
        return dst, b

    def wload_g(self, dram_fn, ng, view):
        i = self.wi % len(self.wbufs)
        self.wi += 1
        t, b = self.wbufs[i]
        dst = view(t)
        for g in range(ng):
            self.S.dma("pool", dst[:, :, g, :], dram_fn(g), writes=[b])
        return dst, b

    def build(self):
        nc, S, d = self.nc, self.S, self.d
        self.xT = self.sb("xT", [128, KC, T], F32)
        self.hT = self.sb("hT", [128, KC, T], BF16)
        self.xTb = [[Buf("x%d_%d" % (c, tb)) for tb in range(NTB)] for c in range(KC)]
        self.hTb = [[Buf("h%d_%d" % (c, tb)) for tb in range(NTB)] for c in range(KC)]
        self.cf = self.sb("cf", [128, NCONST], F32)
        self.cb = self.sb("cbf", [128, 512], BF16)
        self.cfb, self.cbb = Buf("cf"), Buf("cb")
        self.par = self.sb("par", [128, DEPTH, 128], F32)
        self.parb = Buf("par")
        self.wbuf_bufs = [Buf("wb%d" % i) for i in range(6)]
        self.wbufs = []
        self.wi = 0
        self.ps = [(self.psum("ps%d" % i), Buf("ps%d" % i)) for i in range(8)]
        self.psi = 0
        self.lamt = [self.sb("lamt%d" % l, [128, 4, 64], F32) for l in range(DEPTH)]
        self.lampr = [self.sb("lampr%d" % l, [128, 2, 64], F32) for l in range(DEPTH)]
        rem = nc.sbuf_bytes_remaining
        self.arena_elems = (rem - 2048) // 2
        self.arena = self.sb("arena", [128, self.arena_elems], BF16)
        self.aoff = 0

        cf = self.cf
        self.ident_f = cf[:, CI:CI + 128]
        self.U_f = cf[:, CU:CU + 128]
        self.ones_f = cf[:, CO:CO + 128]
        S.dma("sp", cf[:], d["consts"], writes=[self.cfb])
        self.copy("dve", self.cb[:], cf[:, 0:512], [self.cfb], [self.cbb])
        self.ident_b = self.cb[:, CI:CI + 128]
        self.U_b = self.cb[:, CU:CU + 128]
        self.ones_b = self.cb[:, CO:CO + 128]
        self.blk_b = self.cb[:, CB:CB + 128]

        self.epsg = self.sb("epsg", [128, 1], F32)
        S.op("dve", lambda e: e.memset(self.epsg[:], EPS), [], [self.cfb])
        self.load_params()
        self.load_x()
        for l in range(DEPTH):
            self.layer(l)
            if self.stop_after is not None and self.stop_after[0] == l:
                break
        self.store_x()
        S.replay()
        S.close()
        return nc

    def areset(self):
        self.S.barrier()
        self.aoff = 0

    def set_wbufs(self, n):
        self.wbufs = [(self.aalloc([128, 4096], BF16), self.wbuf_bufs[i]) for i in range(n)]
        self.wi = 0

    def aalloc(self, shape, dt):
        n = int(np.prod(shape[1:]))
        if dt == F32:
            if self.aoff % 2:
                self.aoff += 1
            ne = 2 * n
        else:
            ne = n
        assert self.aoff + ne <= self.arena_elems, ("arena overflow", self.aoff, ne, self.arena_elems)
        v = self.arena[:, self.aoff:self.aoff + ne]
        self.aoff += ne
        if dt == F32:
            v = v.bitcast(F32)
        if len(shape) == 3:
            v = v.rearrange("p (a b) -> p a b", a=shape[1])
        elif len(shape) == 4:
            v = v.rearrange("p (a b c) -> p a b c", a=shape[1], b=shape[2])
        return v

    def nps(self, pool=None):
        pool = pool or (0, 1, 2, 3)
        k = getattr(self, "_psk", {})
        i = k.get(pool, 0)
        k[pool] = i + 1
        self._psk = k
        return self.ps[pool[i % len(pool)]]

    PA_NORM = 0
    PA_CW = 24
    PA_CB = 56
    PA_MHN = 64
    PA_AHN = 68
    PA_PSC = 72
    PA_AQ = 76
    PA_AK = 77
    PA_GB = 78
    PA_RB = 86
    PA_LAM = 94
    PA_TMP = 96

    def load_params(self):
        S, d, par, pb = self.S, self.d, self.par, self.parb
        kw = dict(allow_slow_non_contiguous=True, nowait=True)
        for l in range(DEPTH):
            for i, nm in enumerate(("attn_norm", "ffn_norm", "ple_norm")):
                S.dma("act", par[:, l, self.PA_NORM + 8 * i:self.PA_NORM + 8 * i + 8],
                      d[nm][l].rearrange("(c p) -> p c", p=128), writes=[pb], **kw)
            for j in range(4):
                S.dma("act", par[:, l, self.PA_CW + 8 * j:self.PA_CW + 8 * j + 8],
                      d["m_conv_w"][l, j].rearrange("(c p) -> p c", p=128), writes=[pb], **kw)
            S.dma("act", par[:, l, self.PA_CB:self.PA_CB + 8], d["m_conv_b"][l].rearrange("(c p) -> p c", p=128), writes=[pb], **kw)
            S.dma("act", par[:, l, self.PA_MHN:self.PA_MHN + 4], d["m_head_norm"][l].rearrange("(c p) -> p c", p=128), writes=[pb], **kw)
            S.dma("act", par[:, l, self.PA_AHN:self.PA_AHN + 4], d["a_head_norm"][l].rearrange("(c p) -> p c", p=128), writes=[pb], **kw)
            S.dma("act", par[:, l, self.PA_PSC:self.PA_PSC + 4], d["pool_scale"][l].rearrange("(c p) -> p c", p=128), writes=[pb], **kw)
            for half in range(2):
                S.dma("act", par[half * 64:(half + 1) * 64, l, self.PA_AQ:self.PA_AQ + 1],
                      d["a_q_norm"][l].rearrange("(p o) -> p o", o=1), writes=[pb], **kw)
                S.dma("act", par[half * 64:(half + 1) * 64, l, self.PA_AK:self.PA_AK + 1],
                      d["a_k_norm"][l].rearrange("(p o) -> p o", o=1), writes=[pb], **kw)
            S.dma("act", par[:, l, self.PA_GB:self.PA_GB + 8], d["m_gate_bias"][l].partition_broadcast(128), writes=[pb], **kw)
            if l % 2 == 1:
                S.dma("act", par[:, l, self.PA_RB:self.PA_RB + 8], d["router_b"][l // 2].partition_broadcast(128), writes=[pb], **kw)
            lt = self.lamt[l]
            S.dma("act", lt[:], d["a_lambda"][l].partition_broadcast(128), writes=[pb], **kw)
            tmp = par[:, l, self.PA_TMP:self.PA_TMP + 32]
            pr = self.lampr[l]
            self.tt("dve", pr[:, 0, :], lt[:, 0, :], lt[:, 1, :], ALU.mult, [pb], [pb])
            self.tt("dve", pr[:, 1, :], lt[:, 2, :], lt[:, 3, :], ALU.mult, [pb], [pb])
            S.op("dve", lambda e, pr=pr, tmp=tmp: e.reduce_sum(tmp[:, 0:2], pr[:], AX.X), [pb], [pb])
            self.act(tmp[:, 2:4], tmp[:, 0:2], AF.Exp, [pb], [pb])
            lam_init = 0.8 - 0.6 * math.exp(-0.3 * l)
            self.stt("dve", par[:, l, self.PA_LAM:self.PA_LAM + 1], tmp[:, 3:4], -lam_init, tmp[:, 2:3],
                     ALU.add, ALU.subtract, [pb], [pb])
            self.ts("dve", par[:, l, self.PA_AQ:self.PA_AQ + 1], par[:, l, self.PA_AQ:self.PA_AQ + 1], 0.125, None,
                    ALU.mult, None, [pb], [pb])

    def pcol(self, l, off, n=1):
        return self.par[:, l, off:off + n]

    def load_x(self):
        S, d = self.S, self.d
        self.areset()
        xs = [self.aalloc([128, D], F32) for _ in range(2)]
        xsb = [Buf("xs0"), Buf("xs1")]
        for tt in range(NTT):
            s, sbuf = xs[tt % 2], xsb[tt % 2]
            S.dma("sp", s, d["x"][tt * 128:(tt + 1) * 128, :], writes=[sbuf])
            for half in range(2):
                pt, pb = self.nps()
                for i in range(4):
                    c = half * 4 + i
                    self.transpose(pt[:, i * 128:(i + 1) * 128], s[:, c * 128:(c + 1) * 128], self.ident_f,
                                   [sbuf, self.cfb], [pb])
                tb = tt // 4
                self.copy("dve" if half == 0 else "act",
                          self.xT[:, half * 4:half * 4 + 4, tt * 128:(tt + 1) * 128],
                          pt[:].rearrange("p (a b) -> p a b", a=4), [pb],
                          [self.xTb[half * 4 + i][tb] for i in range(4)])

    def store_x(self):
        S = self.S
        self.areset()
        xs = [self.aalloc([128, D], F32) for _ in range(2)]
        xsb = [Buf("os0"), Buf("os1")]
        for tt in range(NTT):
            s, sbuf = xs[tt % 2], xsb[tt % 2]
            tb = tt // 4
            for half in range(2):
                pt, pb = self.nps()
                for i in range(4):
                    c = half * 4 + i
                    self.transpose(pt[:, i * 128:(i + 1) * 128], self.xT[:, c, tt * 128:(tt + 1) * 128], self.ident_f,
                                   [self.xTb[c][tb], self.cfb], [pb])
                self.copy("dve" if half == 0 else "act", s[:, half * 512:(half + 1) * 512], pt[:], [pb], [sbuf])
            S.dma("sp", self.out[tt * 128:(tt + 1) * 128, :], s, reads=[sbuf])
        S.wait_all("sp", xsb + getattr(self, "tapbufs", []))

    def rmsnorm(self, l, which):
        gcol = self.PA_NORM + 8 * which
        pend = {}

        def emit_sq(tb):
            sl = slice(tb * TB, (tb + 1) * TB)
            sq, sqb = self.n_sq[tb % 2], self.n_sqb[tb % 2]
            for c in range(KC):
                if c < 6:
                    self.act(sq[:, c, :], self.xT[:, c, sl], AF.Square, [self.xTb[c][tb]], [sqb[c]])
                else:
                    self.tt("dve", sq[:, c, :], self.xT[:, c, sl], self.xT[:, c, sl], ALU.mult, [self.xTb[c][tb]], [sqb[c]])
            pt, pb = self.nps((6, 7))
            self.mm(pt[:], [(self.ones_b, sq[:, c, :]) for c in range(KC)], sqb + [self.cbb], [pb])
            pend[tb] = (pt, pb)

        def emit_fin(tb):
            sl = slice(tb * TB, (tb + 1) * TB)
            rs, rsb = self.n_rs[tb % 2], self.n_rsb[tb % 2]
            pt, pb = pend.pop(tb)
            self.act(rs[:], pt[:], AF.Ln, [pb, self.cfb], [rsb], bias=self.epsg[:, 0:1], scale=1.0 / D)
            self.act(rs[:], rs[:], AF.Exp, [rsb], [rsb], scale=-0.5)
            for c in range(KC):
                self.stt("dve", self.hT[:, c, sl], self.xT[:, c, sl],
                         self.pcol(l, gcol + c), rs[:], ALU.mult, ALU.mult,
                         [self.xTb[c][tb], rsb, self.parb], [self.hTb[c][tb]])

        emit_sq(0)
        for tb in range(NTB):
            if tb + 1 < NTB:
                emit_sq(tb + 1)
            emit_fin(tb)

    def alloc_norm_tmp(self):
        self.n_sq = [self.aalloc([128, KC, TB], BF16) for _ in range(2)]
        self.n_sqb = [[Buf("sq%d_%d" % (i, c)) for c in range(KC)] for i in range(2)]
        self.n_rs = [self.aalloc([128, TB], F32) for _ in range(2)]
        self.n_rsb = [Buf("rs0"), Buf("rs1")]

    def hreads(self, tb):
        return [self.hTb[c][tb] for c in range(KC)]

    def layer(self, l):
        S = self.S
        st = self.stop_after
        self.areset()
        self.alloc_norm_tmp()
        self.rmsnorm(l, 0)
        self.tap("h%d" % l, self.hT[:], [128, KC, T], BF16, [b for r in self.hTb for b in r])
        if st == (l, "norm1"):
            return
        self.areset()
        self.set_wbufs(2)
        self.zT = self.aalloc([128, KC, T], BF16)
        self.zTb = [[Buf("z%d_%d" % (c, tb)) for tb in range(NTB)] for c in range(KC)]
        self.hb = self.aalloc([128, 4, T], BF16)
        self.hbb = [[Buf("hb%d_%d" % (c, tb)) for tb in range(NTB)] for c in range(4)]
        base2 = self.aoff
        for r, fn in enumerate((self.mlstm, self.dattn, self.poolb)):
            S.barrier()
            self.aoff = base2
            fn(l)
            self.tap(("hm%d", "ha%d", "hp%d")[r] % l, self.hb[:], [128, 4, T], BF16, [b for rr in self.hbb for b in rr])
            if st == (l, ("hm", "ha", "hp")[r]):
                return
            S.barrier()
            self.aoff = base2
            self.merge(l, r)
        self.tap("z%d" % l, self.zT[:], [128, KC, T], BF16, [b for r in self.zTb for b in r])
        self.wout(l)
        if st == (l, "mix"):
            return
        self.areset()
        self.alloc_norm_tmp()
        self.rmsnorm(l, 1)
        self.set_wbufs(6)
        self.alloc_ffn_tmp()
        if l % 2 == 0:
            self.ffn(l, None)
        else:
            self.moe(l)
        if st == (l, "ffn"):
            return
        self.areset()
        self.alloc_norm_tmp()
        self.rmsnorm(l, 2)
        self.set_wbufs(3)
        self.ple(l)

    def proj_fm(self, w, wb, m, rhs_fn, nk, pool=None):
        for tb in range(NTB):
            pt, pb = self.nps(pool)
            pairs, reads = [], [wb]
            for kc in range(nk):
                r, rb = rhs_fn(kc, tb)
                pairs.append((w[:, kc, m * 128:(m + 1) * 128], r))
                reads.append(rb)
            self.mm(pt[:], pairs, reads, [pb])
            yield tb, pt, pb

    def h_rhs(self, kc, tb):
        return self.hT[:, kc, tb * TB:(tb + 1) * TB], self.hTb[kc][tb]

    def proj_tm(self, w_fn, wb, out_v, out_vb, nk=KC):
        for q4 in range(4):
            pt, pb = self.nps()
            for i in range(4):
                tt = q4 * 4 + i
                tb = tt // 4
                pairs = [(self.hT[:, kc, tt * 128:(tt + 1) * 128], w_fn(kc)) for kc in range(nk)]
                self.mm(pt[:, i * 128:(i + 1) * 128], pairs, self.hreads(tb) + [wb], [pb])
            self.copy("act", out_v[:, q4 * 4:q4 * 4 + 4, :], pt[:].rearrange("p (a b) -> p a b", a=4), [pb], [out_vb])

    def mlstm(self, l):
        S, d = self.S, self.d
        w_in = d["w_in"][l]
        wg, wgb = self.wload(w_in[:, C_MI:C_MI + 8].rearrange("(c p) n -> p c n", p=128),
                             lambda t: t[:, 0:64].rearrange("p (c n) -> p c n", c=KC))
        GT = self.aalloc([128, NTT, 8], F32)
        nlf = self.aalloc([128, NTT, 4], F32)
        acol = self.aalloc([128, NTT, 4], F32)
        gb = Buf("gates")
        pt, pb = self.nps((6, 7))
        for tt in range(NTT):
            pairs = [(self.hT[:, kc, tt * 128:(tt + 1) * 128], wg[:, kc, :]) for kc in range(KC)]
            self.mm(pt[:, tt * 8:(tt + 1) * 8], pairs, self.hreads(tt // 4) + [wgb], [pb])
        self.tt("dve", GT[:], pt[:, 0:128].rearrange("p (a b) -> p a b", a=NTT),
                self.pcol(l, self.PA_GB, 8).unsqueeze(1).broadcast_to([128, NTT, 8]), ALU.add, [pb, self.parb], [gb])
        self.act(nlf[:], GT[:, :, 4:8], AF.Exp, [gb], [gb], scale=-1.0)
        self.act(nlf[:], nlf[:], AF.Ln, [gb], [gb], bias=1.0)
        pt, pb = self.nps((6, 7))
        self.mm1(pt[:, 0:64], self.U_f, nlf[:].rearrange("p a b -> p (a b)"), [gb, self.cfb], [pb])
        self.stt("dve", acol[:], pt[:, 0:64].rearrange("p (a b) -> p a b", a=NTT), LNS, GT[:, :, 0:4],
                 ALU.add, ALU.add, [pb, gb], [gb])

        pre1 = self.aalloc([128, T + 4], BF16)
        pre = [pre1, pre1]
        preb1 = Buf("pre")
        preb = [preb1, preb1]
        dgt = self.aalloc([128, 8, 128], BF16)
        dgtb = Buf("dgt")
        qk = [self.aalloc([128, T], BF16) for _ in range(2)]
        qkb = [Buf("q"), Buf("k")]
        vh = self.aalloc([128, NTT, 128], BF16)
        vhb = Buf("vh")
        og = self.aalloc([128, T], BF16)
        ogb = [Buf("og%d" % tb) for tb in range(NTB)]
        NU = self.aalloc([128, 128], BF16); NUb = Buf("NU")
        self.ts("dve", NU[:], self.U_f, -30000.0, 30000.0, ALU.mult, ALU.add, [self.cfb], [NUb])
        rhsb_t = [self.aalloc([128, 128], F32) for _ in range(3)]; rhsbb = [Buf("rhsb%d" % i) for i in range(3)]
        DT = [self.aalloc([128, 128], F32) for _ in range(2)]; DTb = [Buf("DT0"), Buf("DT1")]
        ebt = [self.aalloc([128, 128], F32) for _ in range(3)]; ebb = [Buf("eb%d" % i) for i in range(3)]
        epsc = self.aalloc([128, 2], F32)
        S.op("dve", lambda e: e.memset(epsc[:, 0:1], EPS), [], [self.parb])
        ekl = [self.aalloc([128, 2], F32) for _ in range(2)]; eklb = [Buf("ekl0"), Buf("ekl1")]
        STm = [self.aalloc([128, 128], BF16) for _ in range(2)]; STmb = [Buf("STm0"), Buf("STm1")]
        K2 = [self.aalloc([128, 128], BF16) for _ in range(2)]; K2b = [Buf("K20"), Buf("K21")]
        qp = [self.aalloc([128, 128], BF16) for _ in range(2)]; qpb = [Buf("qp0"), Buf("qp1")]
        Cn = self.aalloc([128, 256], F32); Cnb = Buf("Cn")
        Cbf = self.aalloc([128, 256], BF16); Cbfb = Buf("Cbf")
        tmp = self.aalloc([128, 128], F32); tmpb = Buf("tmp")
        hraw = [self.aalloc([128, TB], BF16) for _ in range(2)]; hrawb = [Buf("hraw0"), Buf("hraw1")]
        sqh = self.aalloc([128, TB], BF16); sqhb = Buf("sqh")
        rsh = self.aalloc([128, TB], F32); rshb = Buf("rsh")
        S.op("dve", lambda e: e.memset(pre1[:, 0:4], 0.0), [], [preb1])

        def load_w4(h):
            return self.wload_g(
                lambda g, h=h: w_in[:, g * 512 + h * 128:g * 512 + (h + 1) * 128].rearrange("(c p) n -> p c n", p=128), 4,
                lambda t: t[:].rearrange("p (c g n) -> p c g n", c=KC, g=4))

        w4n = load_w4(0)
        for h in range(4):
            w4, w4b = w4n
            self.proj_tm(lambda kc: w4[:, kc, 2, :], w4b, vh, vhb)
            for tb in range(NTB):
                pt, pb = self.nps()
                self.mm(pt[:], [(w4[:, kc, 3, :], self.hT[:, kc, tb * TB:(tb + 1) * TB]) for kc in range(KC)],
                        self.hreads(tb) + [w4b], [pb])
                self.act(og[:, tb * TB:(tb + 1) * TB], pt[:], AF.Sigmoid, [pb], [ogb[tb]])
            for j in range(2):
                ch = j * 4 + h
                for tap in range(4):
                    self.ts("dve", dgt[:, j * 4 + tap, :], self.ident_f, self.pcol(l, self.PA_CW + 8 * tap + ch), None,
                            ALU.mult, None, [self.cfb, self.parb], [dgtb])
            for j in range(2):
                ch = j * 4 + h
                for tb in range(NTB):
                    pt, pb = self.nps()
                    self.mm(pt[:], [(w4[:, kc, j, :], self.hT[:, kc, tb * TB:(tb + 1) * TB]) for kc in range(KC)],
                            self.hreads(tb) + [w4b], [pb])
                    self.copy("act" if tb % 2 == 0 else "dve", pre1[:, 3 + tb * TB:3 + (tb + 1) * TB], pt[:], [pb], [preb1])
                for tb in range(NTB):
                    pt, pb = self.nps()
                    self.mm(pt[:], [(dgt[:, j * 4 + tap, :], pre1[:, tb * TB + tap:tb * TB + tap + TB]) for tap in range(4)],
                            [dgtb, preb1], [pb])
                    self.act(qk[j][:, tb * TB:(tb + 1) * TB], pt[:], AF.Silu, [pb, self.parb], [qkb[j]],
                             bias=self.pcol(l, self.PA_CB + ch))
            q, k = qk
            if h + 1 < 4:
                w4n = load_w4(h + 1)
            stA, stB = {}, {}
            norm_pending = []

            def stage_a(c):
                cs = slice(c * 128, (c + 1) * 128)
                r_, rb_ = rhsb_t[c % 3], rhsbb[c % 3]
                self.ts("pool", r_[:], self.U_f, nlf[:, c, h:h + 1], None, ALU.mult, None, [self.cfb, gb], [rb_])
                nb, nbb = self.nps((6, 7))
                self.mm1(nb[:, 0:128], self.ones_f, r_[:], [rb_, self.cfb], [nbb])
                self.mm(nb[:, 128:256], [(self.ones_f, r_[:]), (self.ident_b, NU[:])], [rb_, self.cfb, self.cbb, NUb], [nbb])
                st, stb = self.nps((0, 1))
                self.mm1(st[:, 0:128], k[:, cs], q[:, cs], [qkb[0], qkb[1]], [stb])
                trv = None
                if c < NTT - 1:
                    trv = st[:].bitcast(BF16)[:, 512:640]
                    self.transpose(trv, k[:, cs], self.ident_b, [qkb[1], self.cbb], [stb])
                stA[c] = (nb, nbb, st, stb, trv)

            def stage_b(c):
                cs = slice(c * 128, (c + 1) * 128)
                nb, nbb, st, stb, trv = stA.pop(c)
                i2, i3 = c % 2, c % 3
                self.act(DT[i2][:], nb[:, 128:256], AF.Exp, [nbb, gb], [DTb[i2]], bias=acol[:, c, h:h + 1], scale=-1.0)
                self.act(ebt[i3][:], nb[:, 0:128], AF.Exp, [nbb], [ebb[i3]], scale=-1.0)
                self.tt("dve", STm[i2][:], st[:, 0:128], DT[i2][:], ALU.mult, [stb, DTb[i2]], [STmb[i2]])
                if c < NTT - 1:
                    self.act(ekl[i2][:, 0:1], nb[:, 127:128], AF.Exp, [nbb, gb], [eklb[i2]], bias=acol[:, c, h:h + 1],
                             scale=-1.0)
                    self.ts("dve", K2[i2][:], trv, ekl[i2][:, 0:1], None, ALU.mult, None, [stb, eklb[i2]], [K2b[i2]])
                if c > 0:
                    self.tt("pool", qp[i2][:], q[:, cs], ebt[i3][:], ALU.mult, [qkb[0], ebb[i3]], [qpb[i2]])

            def stage_c(c):
                cs = slice(c * 128, (c + 1) * 128)
                i2, i3 = c % 2, c % 3
                if c < NTT - 1:
                    kv, kvb = self.ps[2]
                    self.mm1(kv[:, 0:128], K2[i2][:], vh[:, c, :], [K2b[i2], vhb], [kvb])
                    self.mm1(kv[:, 128:256], K2[i2][:], self.ones_b, [K2b[i2], self.cbb], [kvb])
                nd, ndb = self.nps((4, 5))
                last = (c == 0)
                self.mm1(nd[:, 0:128], vh[:, c, :], STm[i2][:], [vhb, STmb[i2]], [ndb], start=True, stop=last)
                if c > 0:
                    self.mm1(nd[:, 0:128], Cbf[:, 0:128], qp[i2][:], [Cbfb, qpb[i2]], [ndb], start=False, stop=True)
                self.mm1(nd[:, 128:256], self.ones_b, STm[i2][:], [self.cbb, STmb[i2]], [ndb], start=True, stop=last)
                if c > 0:
                    self.mm1(nd[:, 128:256], Cbf[:, 128:256], qp[i2][:], [Cbfb, qpb[i2]], [ndb], start=False, stop=True)
                if c < NTT - 1:
                    if c == 0:
                        self.copy("dve", Cn[:], kv[:, 0:256], [kvb], [Cnb])
                    else:
                        self.stt("dve", Cn[:], Cn[:], ebt[i3][:, 127:128], kv[:, 0:256], ALU.mult, ALU.add,
                                 [Cnb, ebb[i3], kvb], [Cnb])
                    self.copy("act", Cbf[:], Cn[:], [Cnb], [Cbfb])
                self.ts("dve", tmp[:], nd[:, 128:256], -1.0, 1.0, ALU.mult, ALU.max, [ndb], [tmpb])
                self.tt("dve", tmp[:], tmp[:], nd[:, 128:256], ALU.max, [tmpb, ndb], [tmpb])
                S.op("dve", lambda e: e.reciprocal(tmp[:], tmp[:]), [tmpb], [tmpb])
                ci = c % 4
                hi = (c // 4) % 2
                self.tt("dve", hraw[hi][:, ci * 128:(ci + 1) * 128], nd[:, 0:128], tmp[:], ALU.mult, [ndb, tmpb], [hrawb[hi]])
                if ci == 3:
                    self.tt("pool", sqh[:], hraw[hi][:], hraw[hi][:], ALU.mult, [hrawb[hi]], [sqhb])
                    norm_pending.append(c)

            def norm_tail(c):
                tb = c // 4
                hi = tb % 2
                ss, ssb = self.ps[3]
                self.mm1(ss[:], self.ones_b, sqh[:], [self.cbb, sqhb], [ssb])
                self.act(rsh[:], ss[:], AF.Ln, [ssb, self.parb], [rshb], bias=epsc[:, 0:1], scale=1.0 / 128)
                self.act(rsh[:], rsh[:], AF.Exp, [rshb], [rshb], scale=-0.5)
                self.tt("dve", rsh[:], hraw[hi][:], rsh[:], ALU.mult, [hrawb[hi], rshb], [rshb])
                self.stt("dve", self.hb[:, h, tb * TB:(tb + 1) * TB], rsh[:], self.pcol(l, self.PA_MHN + h),
                         og[:, tb * TB:(tb + 1) * TB], ALU.mult, ALU.mult, [rshb, self.parb, ogb[tb]],
                         [self.hbb[h][tb]])

            stage_a(0)
            stage_a(1)
            stage_b(0)
            for c in range(NTT):
                if c + 2 < NTT:
                    stage_a(c + 2)
                if c + 1 < NTT:
                    stage_b(c + 1)
                if norm_pending:
                    norm_tail(norm_pending.pop(0))
                stage_c(c)
            while norm_pending:
                norm_tail(norm_pending.pop(0))

    def dattn(self, l):
        S, d = self.S, self.d
        w_in = d["w_in"][l]
        lam_init = 0.8 - 0.6 * math.exp(-0.3 * l)
        qn = self.aalloc([128, T], BF16); qnb = [Buf("qn%d" % tb) for tb in range(NTB)]
        knt = self.aalloc([128, TB], BF16); kntb = Buf("knt")
        knz = [self.aalloc([128, T], BF16) for _ in range(2)]
        knb = [Buf("kn%d" % tb) for tb in range(NTB)]
        kn = None
        vh = self.aalloc([128, NTT, 128], BF16); vhb = Buf("vha")
        raw = [self.aalloc([128, TB], F32) for _ in range(2)]; rawb = [Buf("raw0"), Buf("raw1")]
        sq = [self.aalloc([128, TB], BF16) for _ in range(2)]; sqb = [Buf("sqa0"), Buf("sqa1")]
        rs = [self.aalloc([128, TB], F32) for _ in range(2)]; rsb = [Buf("rsa0"), Buf("rsa1")]
        NEG = self.aalloc([128, 128], BF16); NEGb = Buf("NEG")
        self.ts("dve", NEG[:], self.U_f, 30000.0, -30000.0, ALU.mult, ALU.add, [self.cfb], [NEGb])
        P = [self.aalloc([128, 2, QB], BF16) for _ in range(3)]
        Pb = [Buf("P0"), Buf("P1"), Buf("P2")]
        self.epsc = self.aalloc([128, 2], F32)
        S.op("dve", lambda e: e.memset(self.epsc[:, 0:1], EPS), [], [self.parb])
        S.op("dve", lambda e: e.memset(self.epsc[:, 1:2], EPS / (1.0 - lam_init) ** 2), [], [self.parb])
        rden = self.aalloc([128, 2 * QB], F32); rdenb = Buf("rden")
        tq = self.aalloc([128, 2 * QB], F32); tqb = Buf("tq")
        od = [self.aalloc([128, QB], F32) for _ in range(2)]; odb = [Buf("od0"), Buf("od1")]
        sqo = [self.aalloc([128, QB], BF16) for _ in range(2)]; sqob = [Buf("sqo0"), Buf("sqo1")]
        rso = self.aalloc([128, QB], F32); rsob = Buf("rso")
        pi = 0
        def load_w3(h):
            return self.wload_g(
                lambda g, h=h: w_in[:, C_AQ + g * 512 + h * 128:C_AQ + g * 512 + (h + 1) * 128].rearrange(
                    "(c p) n -> p c n", p=128), 3,
                lambda t: t[:, 0:3072].rearrange("p (c g n) -> p c g n", c=KC, g=3))

        w3n = load_w3(0)
        for h in range(4):
            w3, w3b = w3n
            items = [(tb, j) for tb in range(NTB) for j in range(2)]

            def qk_stage1(n):
                tb, j = items[n]
                raw_, rawb_ = raw[n % 2], rawb[n % 2]
                sq_, sqb_ = sq[n % 2], sqb[n % 2]
                pt, pb = self.nps()
                self.mm(pt[:], [(w3[:, kc, j, :], self.hT[:, kc, tb * TB:(tb + 1) * TB]) for kc in range(KC)],
                        self.hreads(tb) + [w3b], [pb])
                self.copy("act", raw_[:], pt[:], [pb], [rawb_])
                self.tt("pool" if n % 2 else "dve", sq_[:], raw_[:], raw_[:], ALU.mult, [rawb_], [sqb_])

            def qk_stage2(n):
                tb, j = items[n]
                raw_, rawb_ = raw[n % 2], rawb[n % 2]
                sq_, sqb_ = sq[n % 2], sqb[n % 2]
                rs_, rsb_ = rs[n % 2], rsb[n % 2]
                gc = self.PA_AQ if j == 0 else self.PA_AK
                ss, ssb = self.nps((6, 7))
                self.mm1(ss[:], self.blk_b, sq_[:], [self.cbb, sqb_], [ssb])
                self.act(rs_[:], ss[:], AF.Ln, [ssb, self.parb], [rsb_], bias=self.epsc[:, 0:1], scale=1.0 / 64)
                self.act(rs_[:], rs_[:], AF.Exp, [rsb_], [rsb_], scale=-0.5)
                if j == 0:
                    self.stt("dve", qn[:, tb * TB:(tb + 1) * TB], raw_[:], self.pcol(l, gc), rs_[:], ALU.mult, ALU.mult,
                             [rawb_, rsb_, self.parb], [qnb[tb]])
                else:
                    self.stt("dve", knt[:], raw_[:], self.pcol(l, gc), rs_[:], ALU.mult, ALU.mult,
                             [rawb_, rsb_, self.parb], [kntb])
                    for m in range(2):
                        self.ts("pool" if m else "dve", knz[m][:, tb * TB:(tb + 1) * TB], knt[:],
                                self.cf[:, CB + 64 * m:CB + 64 * m + 1], None, ALU.mult, None,
                                [kntb, self.cfb], [knb[tb]])

            qk_stage1(0)
            for n in range(len(items)):
                if n + 1 < len(items):
                    qk_stage1(n + 1)
                qk_stage2(n)
            self.proj_tm(lambda kc: w3[:, kc, 2, :], w3b, vh, vhb)
            blocks = []
            for qb in range(T // QB):
                nkt = 2 * qb + 2
                for kt in range(nkt):
                    blocks.append((qb, kt, nkt))
            accp = [(self.ps[3], self.ps[4]), (self.ps[5], self.ps[6])]
            state = {}

            def emit_S(i):
                qb, kt, nkt = blocks[i]
                q0, k0 = qb * QB, kt * 128
                lo = 128 if kt == nkt - 1 else 0
                diag = kt >= nkt - 2
                sp_, spb = self.nps((0, 1, 2))
                for m in range(2):
                    self.mm1(sp_[:, m * QB + lo:(m + 1) * QB], knz[m][:, k0:k0 + 128],
                             qn[:, q0 + lo:q0 + QB], [knb[k0 // TB], qnb[q0 // TB]], [spb], start=True, stop=not diag)
                    if diag:
                        self.mm1(sp_[:, m * QB + lo:m * QB + lo + 128], self.ident_b, NEG[:], [self.cbb, NEGb], [spb],
                                 start=False, stop=True)
                state[i] = (sp_, spb)

            def emit_rest(i):
                qb, kt, nkt = blocks[i]
                q0, k0 = qb * QB, kt * 128
                tbq = q0 // TB
                lo = 128 if kt == nkt - 1 else 0
                diag = kt >= nkt - 2
                di = (q0 - k0) // 128 + 1
                sp_, spb = state.pop(i)
                (ops, opb), (dps, dpb) = accp[qb % 2]
                Pt, Ptb = P[i % 3], Pb[i % 3]
                spv = sp_[:].rearrange("p (a b) -> p a b", a=2)
                self.act(Pt[:, :, lo:QB], spv[:, :, lo:QB], AF.Exp, [spb, self.cfb], [Ptb],
                         bias=self.cf[:, CAL + h * 16 + di:CAL + h * 16 + di + 1])
                first = kt == 0
                if lo == 0:
                    self.mm1(ops[:], vh[:, kt, :], Pt[:].rearrange("p a b -> p (a b)"), [vhb, Ptb], [opb],
                             start=first, stop=False)
                    self.mm1(dps[:], self.ones_b, Pt[:].rearrange("p a b -> p (a b)"), [self.cbb, Ptb], [dpb],
                             start=first, stop=False)
                else:
                    for m in range(2):
                        self.mm1(ops[:, m * QB + lo:(m + 1) * QB], vh[:, kt, :], Pt[:, m, lo:QB], [vhb, Ptb], [opb],
                                 start=False, stop=True)
                        self.mm1(dps[:, m * QB + lo:(m + 1) * QB], self.ones_b, Pt[:, m, lo:QB], [self.cbb, Ptb], [dpb],
                                 start=False, stop=True)
                if kt == nkt - 1:
                    S.op("dve", lambda e, dps=dps: e.reciprocal(rden[:], dps[:]), [dpb], [rdenb])
                    self.tt("dve", tq[:], ops[:], rden[:], ALU.mult, [opb, rdenb], [tqb])
                    odt, odtb = od[qb % 2], odb[qb % 2]
                    self.stt("dve", odt[:], tq[:, QB:2 * QB], self.pcol(l, self.PA_LAM), tq[:, 0:QB], ALU.mult, ALU.add,
                             [tqb, self.parb], [odtb])
                    self.tt("pool", sqo[qb % 2][:], odt[:], odt[:], ALU.mult, [odtb], [sqob[qb % 2]])
                    pending.append((i + 6, qb))

            def emit_post_b(qb):
                q0 = qb * QB
                tbq = q0 // TB
                odt, odtb = od[qb % 2], odb[qb % 2]
                ss, ssb = self.ps[7]
                self.mm1(ss[:, 0:QB], self.ones_b, sqo[qb % 2][:], [self.cbb, sqob[qb % 2]], [ssb])
                sc = 1.0 / (1.0 - lam_init) ** 2
                self.act(rso[:], ss[:, 0:QB], AF.Ln, [ssb, self.parb], [rsob], bias=self.epsc[:, 1:2], scale=sc / 128)
                self.act(rso[:], rso[:], AF.Exp, [rsob], [rsob], scale=-0.5)
                self.stt("dve", self.hb[:, h, q0:q0 + QB], odt[:], self.pcol(l, self.PA_AHN + h), rso[:], ALU.mult,
                         ALU.mult, [odtb, rsob, self.parb], [self.hbb[h][tbq]])

            pending = []
            if h + 1 < 4:
                w3n = load_w3(h + 1)
            emit_S(0)
            emit_S(1)
            for i in range(len(blocks)):
                if i + 2 < len(blocks):
                    emit_S(i + 2)
                emit_rest(i)
                while pending and pending[0][0] <= i:
                    emit_post_b(pending.pop(0)[1])
            while pending:
                emit_post_b(pending.pop(0)[1])

    def poolb(self, l):
        S, d = self.S, self.d
        w_in = d["w_in"][l]
        PAD = 16
        ub = self.aalloc([128, PAD + T], F32); ubb = Buf("u")
        A = self.aalloc([128, PAD + T], F32); Ab = Buf("A")
        Bt = self.aalloc([128, PAD + T], F32); Bb = Buf("B")
        pl = self.aalloc([128, T], BF16); plb = Buf("pl")
        for t_, b_ in ((ub, ubb), (A, Ab), (Bt, Bb)):
            S.op("pool", lambda e, t_=t_: e.memset(t_[:, 0:PAD], 0.0), [], [b_])
        w, wb = self.wload(w_in[:, C_PU:C_PU + 512].rearrange("(c p) n -> p c n", p=128),
                           lambda t: t[:].rearrange("p (c n) -> p c n", c=KC))
        pw, pwb = self.wload(d["pool_w"][l].rearrange("g c d -> c g d"),
                             lambda t: t[:, 0:512].rearrange("p (g d) -> p g d", g=4))
        for g in range(4):
            for tb, pt, pb in self.proj_fm(w, wb, g, self.h_rhs, KC):
                self.copy("act", ub[:, PAD + tb * TB:PAD + (tb + 1) * TB], pt[:], [pb], [ubb])
            src, srcb = ub, ubb
            bufs = [(A, Ab), (Bt, Bb)]
            for step in range(g + 1):
                sh = 1 << step
                dst, dstb = bufs[step % 2]
                self.tt("dve" if step % 2 == 0 else "pool", dst[:, PAD:PAD + T], src[:, PAD:PAD + T],
                        src[:, PAD - sh:PAD - sh + T], ALU.add, [srcb], [dstb])
                src, srcb = dst, dstb
            wn = PWIN[g]
            self.tt("dve", src[:, PAD:PAD + 16], src[:, PAD:PAD + 16], self.cf[:, CPC + g * 16:CPC + g * 16 + 16],
                    ALU.mult, [srcb, self.cfb], [srcb])
            self.stt("dve", pl[:], src[:, PAD:PAD + T], 1.0 / wn, ub[:, PAD:PAD + T], ALU.mult, ALU.subtract,
                     [srcb, ubb], [plb])
            for tb in range(NTB):
                pt, pb = self.nps()
                self.mm1(pt[:], pw[:, g, :], pl[:, tb * TB:(tb + 1) * TB], [pwb, plb], [pb])
                self.act(self.hb[:, g, tb * TB:(tb + 1) * TB], pt[:], AF.Copy, [pb, self.parb], [self.hbb[g][tb]],
                         scale=self.pcol(l, self.PA_PSC + g))

    def merge(self, l, r):
        d = self.d
        w_in = d["w_in"][l]
        gate = [self.aalloc([128, TB], BF16) for _ in range(2)]
        gateb = [Buf("gate0"), Buf("gate1")]
        prod = [self.aalloc([128, TB], BF16) for _ in range(2)]
        prodb = [Buf("prod0"), Buf("prod1")]
        gi = 0
        for cg in range(2):
            c0 = C_G + r * D + cg * 512
            wg, wgb = self.wload(w_in[:, c0:c0 + 512].rearrange("(c p) n -> p c n", p=128),
                                 lambda t: t[:].rearrange("p (c n) -> p c n", c=KC))
            wbr, wbrb = self.wload(d["w_branch"][l, r][:, cg * 512:(cg + 1) * 512].rearrange("(c p) n -> p c n", p=128),
                                   lambda t: t[:, 0:2048].rearrange("p (c n) -> p c n", c=4))
            for m in range(4):
                c = cg * 4 + m
                for tb in range(NTB):
                    sl = slice(tb * TB, (tb + 1) * TB)
                    g_, gb_ = gate[gi % 2], gateb[gi % 2]
                    p_, pb_ = prod[gi % 2], prodb[gi % 2]
                    gi += 1
                    pt, pb = self.nps()
                    self.mm(pt[:], [(wg[:, kc, m * 128:(m + 1) * 128], self.hT[:, kc, sl]) for kc in range(KC)],
                            self.hreads(tb) + [wgb], [pb])
                    self.act(g_[:], pt[:], AF.Sigmoid, [pb], [gb_])
                    pt2, pb2 = self.nps()
                    self.mm(pt2[:], [(wbr[:, kc, m * 128:(m + 1) * 128], self.hb[:, kc, sl]) for kc in range(4)],
                            [self.hbb[kc][tb] for kc in range(4)] + [wbrb], [pb2])
                    if r == 0:
                        self.tt("dve", self.zT[:, c, sl], pt2[:], g_[:], ALU.mult, [pb2, gb_], [self.zTb[c][tb]])
                    else:
                        self.tt("dve", p_[:], pt2[:], g_[:], ALU.mult, [pb2, gb_], [pb_])
                        self.tt("dve", self.zT[:, c, sl], self.zT[:, c, sl], p_[:], ALU.add,
                                [self.zTb[c][tb], pb_], [self.zTb[c][tb]])

    def wout(self, l):
        d = self.d
        for cg in range(2):
            w, wb = self.wload(d["w_out"][l][:, cg * 512:(cg + 1) * 512].rearrange("(c p) n -> p c n", p=128),
                               lambda t: t[:].rearrange("p (c n) -> p c n", c=KC))
            for m in range(4):
                c = cg * 4 + m
                for tb in range(NTB):
                    sl = slice(tb * TB, (tb + 1) * TB)
                    pt, pb = self.nps()
                    self.mm(pt[:], [(w[:, kc, m * 128:(m + 1) * 128], self.zT[:, kc, sl]) for kc in range(KC)],
                            [self.zTb[kc][tb] for kc in range(KC)] + [wb], [pb])
                    self.tt("dve", self.xT[:, c, sl], pt[:], self.xT[:, c, sl], ALU.add, [pb, self.xTb[c][tb]],
                            [self.xTb[c][tb]])

    def ffn(self, l, expert, cbrow=None, cbrowb=None):
        d = self.d
        if expert is None:
            wgu = d["dense_w_gu"][l // 2]
            wdn = d["dense_w_down"][l // 2]
            dff = DFF
        else:
            wgu = d["moe_w_gu"][l // 2, expert]
            wdn = d["moe_w_down"][l // 2, expert]
            dff = DFE
        nj = dff // 128
        a, ab = self.f_a, self.f_ab
        sg, sgb = self.f_sg, self.f_sgb
        si = 0
        j0 = 0
        while j0 < nj:
            n = min(4, nj - j0)
            wg_, wgb_ = self.wload(wgu[:, j0 * 128:(j0 + n) * 128].rearrange("(c p) n -> p c n", p=128),
                                   lambda t: t[:, 0:KC * n * 128].rearrange("p (c n) -> p c n", c=KC))
            wu_, wub_ = self.wload(wgu[:, dff + j0 * 128:dff + (j0 + n) * 128].rearrange("(c p) n -> p c n", p=128),
                                   lambda t: t[:, 0:KC * n * 128].rearrange("p (c n) -> p c n", c=KC))
            wd_, wdb_ = self.wload(wdn[j0 * 128:(j0 + n) * 128, :].rearrange("(j p) n -> p j n", p=128),
                                   lambda t: t[:, 0:n * D].rearrange("p (j n) -> p j n", j=n))
            for jj in range(n):
                for tb in range(NTB):
                    sl = slice(tb * TB, (tb + 1) * TB)
                    s_, sb_ = sg[si % 2], sgb[si % 2]
                    si += 1
                    pg, pgb = self.nps()
                    self.mm(pg[:], [(wg_[:, kc, jj * 128:(jj + 1) * 128], self.hT[:, kc, sl]) for kc in range(KC)],
                            self.hreads(tb) + [wgb_], [pgb])
                    pu, pub = self.nps()
                    self.mm(pu[:], [(wu_[:, kc, jj * 128:(jj + 1) * 128], self.hT[:, kc, sl]) for kc in range(KC)],
                            self.hreads(tb) + [wub_], [pub])
                    self.act(s_[:], pg[:], AF.Silu, [pgb], [sb_])
                    if cbrow is not None:
                        self.tt("pool", s_[:], s_[:], cbrow[:, sl], ALU.mult, [sb_, cbrowb], [sb_])
                    self.tt("dve", a[:, jj, sl], pu[:], s_[:], ALU.mult, [pub, sb_], [ab[jj][tb]])
            for c in range(KC):
                for tb in range(NTB):
                    sl = slice(tb * TB, (tb + 1) * TB)
                    pt, pb = self.nps()
                    self.mm(pt[:], [(wd_[:, jj, c * 128:(c + 1) * 128], a[:, jj, sl]) for jj in range(n)],
                            [ab[jj][tb] for jj in range(n)] + [wdb_], [pb])
                    self.tt("dve", self.xT[:, c, sl], pt[:], self.xT[:, c, sl], ALU.add, [pb, self.xTb[c][tb]],
                            [self.xTb[c][tb]])
            j0 += n

    def alloc_ffn_tmp(self):
        self.f_a = self.aalloc([128, 4, T], BF16)
        self.f_ab = [[Buf("a%d_%d" % (j, tb)) for tb in range(NTB)] for j in range(4)]
        self.f_sg = [self.aalloc([128, TB], BF16) for _ in range(2)]
        self.f_sgb = [Buf("sg0"), Buf("sg1")]

    def moe(self, l):
        S, d = self.S, self.d
        wr, wrb = self.wload(d["router_w"][l // 2].rearrange("(c p) n -> p c n", p=128),
                             lambda t: t[:, 0:64].rearrange("p (c n) -> p c n", c=KC))
        L = self.aalloc([128, NTT, NE], F32); Lb = Buf("L")
        L2 = self.aalloc([128, NTT, NE], F32)
        m1 = self.aalloc([128, NTT], F32)
        m2 = self.aalloc([128, NTT], F32)
        cmb = self.aalloc([128, NTT, NE], F32)
        pt, pb = self.nps((6, 7))
        for tt in range(NTT):
            pairs = [(self.hT[:, kc, tt * 128:(tt + 1) * 128], wr[:, kc, :]) for kc in range(KC)]
            self.mm(pt[:, tt * 8:(tt + 1) * 8], pairs, self.hreads(tt // 4) + [wrb], [pb])
        self.tt("dve", L[:], pt[:, 0:128].rearrange("p (a b) -> p a b", a=NTT),
                self.pcol(l, self.PA_RB, 8).unsqueeze(1).broadcast_to([128, NTT, NE]), ALU.add, [pb, self.parb], [Lb])
        bc = lambda t: t[:].unsqueeze(2).broadcast_to([128, NTT, NE])
        S.op("dve", lambda e: e.tensor_reduce(m1[:], L[:], AX.X, ALU.max), [Lb], [Lb])
        self.tt("dve", L2[:], L[:], bc(m1), ALU.is_equal, [Lb], [Lb])
        self.stt("dve", L2[:], L2[:], -1e30, L[:], ALU.mult, ALU.add, [Lb], [Lb])
        S.op("dve", lambda e: e.tensor_reduce(m2[:], L2[:], AX.X, ALU.max), [Lb], [Lb])
        self.tt("dve", L2[:], L[:], bc(m2), ALU.is_ge, [Lb], [Lb])
        self.tt("dve", cmb[:], L[:], bc(m1), ALU.subtract, [Lb], [Lb])
        self.act(cmb[:], cmb[:], AF.Exp, [Lb], [Lb])
        self.tt("dve", cmb[:], cmb[:], L2[:], ALU.mult, [Lb], [Lb])
        self.tt("dve", m2[:], m2[:], m1[:], ALU.subtract, [Lb], [Lb])
        self.act(m2[:], m2[:], AF.Exp, [Lb], [Lb])
        self.ts("dve", m2[:], m2[:], 1.0, None, ALU.add, None, [Lb], [Lb])
        S.op("dve", lambda e: e.reciprocal(m2[:], m2[:]), [Lb], [Lb])
        self.tt("dve", cmb[:], cmb[:], bc(m2), ALU.mult, [Lb], [Lb])
        self.tap("cmb", cmb[:], [128, NTT, NE], F32, [Lb])
        cbrow = [self.aalloc([128, T], BF16) for _ in range(2)]
        cbrowb = [Buf("cbrow0"), Buf("cbrow1")]
        dg = self.aalloc([128, 128], F32); dgb = Buf("dg")
        for e_ in range(NE):
            cr, crb = cbrow[e_ % 2], cbrowb[e_ % 2]
            for q4 in range(4):
                pt, pb = self.nps((6, 7))
                for i in range(4):
                    tt = q4 * 4 + i
                    self.ts("dve", dg[:], self.ident_f, cmb[:, tt, e_:e_ + 1], None, ALU.mult, None, [self.cfb, Lb], [dgb])
                    self.mm1(pt[:, i * 128:(i + 1) * 128], self.ones_f, dg[:], [self.cfb, dgb], [pb])
                self.copy("act", cr[:, q4 * TB:(q4 + 1) * TB], pt[:], [pb], [crb])
            self.ffn(l, e_, cr, crb)

    def ple(self, l):
        S, d = self.S, self.d
        pT = self.aalloc([128, 2, T], BF16)
        pTb = [Buf("pT%d" % tb) for tb in range(NTB)]
        stg = [self.aalloc([128, PLE], F32) for _ in range(2)]
        stgb = [Buf("pst0"), Buf("pst1")]
        pgt = [self.aalloc([128, TB], F32) for _ in range(2)]
        pgb_ = [Buf("pg0"), Buf("pg1")]
        for tt in range(NTT):
            s, sb_ = stg[tt % 2], stgb[tt % 2]
            S.dma("sp", s, d["p"][l, tt * 128:(tt + 1) * 128, :], writes=[sb_])
            pt, pb = self.nps()
            for i in range(2):
                self.transpose(pt[:, i * 128:(i + 1) * 128], s[:, i * 128:(i + 1) * 128], self.ident_f, [sb_, self.cfb], [pb])
            self.copy("act", pT[:, :, tt * 128:(tt + 1) * 128], pt[:, 0:256].rearrange("p (a b) -> p a b", a=2), [pb],
                      [pTb[tt // 4]])
        gi = 0
        for cg in range(2):
            wg, wgb = self.wload(d["ple_w_gate"][l][:, cg * 512:(cg + 1) * 512].rearrange("(c p) n -> p c n", p=128),
                                 lambda t: t[:].rearrange("p (c n) -> p c n", c=KC))
            wp, wpb = self.wload(d["ple_w_proj"][l][:, cg * 512:(cg + 1) * 512].rearrange("(c p) n -> p c n", p=128),
                                 lambda t: t[:, 0:1024].rearrange("p (c n) -> p c n", c=2))
            for m in range(4):
                c = cg * 4 + m
                for tb in range(NTB):
                    sl = slice(tb * TB, (tb + 1) * TB)
                    g_, gb_ = pgt[gi % 2], pgb_[gi % 2]
                    gi += 1
                    pt, pb = self.nps()
                    self.mm(pt[:], [(wg[:, kc, m * 128:(m + 1) * 128], self.hT[:, kc, sl]) for kc in range(KC)],
                            self.hreads(tb) + [wgb], [pb])
                    self.act(g_[:], pt[:], AF.Sigmoid, [pb], [gb_])
                    pt2, pb2 = self.nps()
                    self.mm(pt2[:], [(wp[:, kc, m * 128:(m + 1) * 128], pT[:, kc, sl]) for kc in range(2)],
                            [pTb[tb], wpb], [pb2])
                    self.tt("dve", g_[:], pt2[:], g_[:], ALU.mult, [pb2, gb_], [gb_])
                    self.tt("dve", self.xT[:, c, sl], self.xT[:, c, sl], g_[:], ALU.add, [self.xTb[c][tb], gb_],
                            [self.xTb[c][tb]])


_INPUT_NAMES = ["attn_norm", "w_in", "m_conv_w", "m_conv_b", "m_gate_bias", "m_head_norm", "a_q_norm", "a_k_norm",
                "a_lambda", "a_head_norm", "pool_w", "pool_scale", "w_branch", "w_out", "ffn_norm", "dense_w_gu",
                "dense_w_down", "router_w", "router_b", "moe_w_gu", "moe_w_down", "ple_norm", "ple_w_gate", "ple_w_proj"]


def make_in_maps(inputs, cores):
    consts = make_consts()
    maps = []
    for b in cores:
        m = {"x": np.ascontiguousarray(inputs["x"][b], dtype=np.float32),
             "p": np.ascontiguousarray(inputs["p"][:, b], dtype=np.float32),
             "consts": consts}
        for k in _INPUT_NAMES:
            m[k] = np.ascontiguousarray(inputs[k], dtype=np.float32)
        maps.append(m)
    return maps


def kernel(**inputs):
    prog = Prog()
    nc = prog.build()
    in_maps = make_in_maps(inputs, list(range(8)))
    res = run_bass_kernel_spmd(nc, in_maps, core_ids=list(range(8)))
    out = np.stack([np.asarray(r["out"], dtype=np.float32) for r in res.results], axis=0)
    return out
```

```python
import math
import numpy as np
import concourse.bass as bass
import concourse.mybir as mybir
from concourse.bass_utils import run_bass_kernel_spmd

F32 = mybir.dt.float32
BF16 = mybir.dt.bfloat16
AF = mybir.ActivationFunctionType
ALU = mybir.AluOpType
AX = mybir.AxisListType

ENGS = ("pe", "act", "dve", "pool", "sp")

D = 1024
T = 2048
DEPTH = 2
PLE = 256
EPS = 1e-6
KC = 8
TB = 512
NTB = 4
NTT = 16
IN_W = 7176
C_MQK, C_MV, C_MO, C_MI, C_MF = 0, 1024, 1536, 2048, 2052
C_AQ, C_AK, C_AV, C_PU, C_G = 2056, 2568, 3080, 3592, 4104
DFF = 2816
NE = 8
DFE = 3584
SLOPES = [2.0 ** (-8.0 * (h + 1) / 4) for h in range(4)]
PWIN = (2, 4, 8, 16)
QB = 256
LNS = math.log(128 ** -0.5)

CI, CU, CO, CB, CAL, CPC, NCONST = 0, 128, 256, 384, 512, 576, 640


def make_consts():
    c = np.zeros((128, NCONST), np.float32)
    p = np.arange(128)
    c[:, CI:CI + 128] = np.eye(128)
    c[:, CU:CU + 128] = (p[:, None] <= p[None, :])
    c[:, CO:CO + 128] = 1.0
    c[:, CB:CB + 128] = (p[:, None] // 64 == p[None, :] // 64)
    for h in range(4):
        for di in range(16):
            c[:, CAL + h * 16 + di] = SLOPES[h] * (p + 128.0 * (1 - di) - (QB - 1))
    for g, w in enumerate(PWIN):
        for t in range(16):
            c[:, CPC + g * 16 + t] = w / min(t + 1, w)
    return c


class Buf:
    __slots__ = ("name", "writer", "readers", "wsem", "rsem")

    def __init__(self, name=""):
        self.name = name
        self.writer = None
        self.readers = {}
        self.wsem = None
        self.rsem = None


class Sched:
    def __init__(self, nc, same_engine_sync=True):
        self.nc = nc
        self.ops = {e: [] for e in ENGS}
        self.cnt = {e: 0 for e in ENGS}
        self.seen = {e: {} for e in ENGS}
        self.sems = {}
        self.same_engine_sync = same_engine_sync
        import os as _os
        self.nosync_engs = set(_os.environ.get("NOSYNC", "pe").split(","))
        self.semcnt = {}
        self._stack = []
        for e in ENGS:
            self.sems[e] = self._alloc("s_" + e)
        self.nsem = len(ENGS)

    def _alloc(self, name):
        cm = self.nc.semaphore(name)
        h = cm.__enter__()
        self._stack.append(cm)
        return h

    def new_sem(self, name):
        key = "d%d_%s" % (self.nsem, name)
        self.nsem += 1
        self.sems[key] = self._alloc(key)
        return key

    def _waits(self, eng, reads, writes):
        need = {}

        def add(ev):
            if ev is None:
                return
            k, v = ev
            if k == eng and (not self.same_engine_sync or eng in self.nosync_engs):
                return
            if k in self.semcnt:
                v = self.semcnt[k]
            if need.get(k, 0) < v:
                need[k] = v

        for b in reads:
            add(b.writer)
        for b in writes:
            add(b.writer)
            for k, v in b.readers.items():
                add((k, v))
        out = []
        seen = self.seen[eng]
        for k, v in need.items():
            if seen.get(k, 0) < v:
                seen[k] = v
                out.append((k, v))
        return out

    def _mark(self, ev, reads, writes):
        k, v = ev
        for b in reads:
            if b.readers.get(k, 0) < v:
                b.readers[k] = v
        for b in writes:
            b.writer = ev
            b.readers = {}

    def op(self, eng, fn, reads=(), writes=()):
        waits = self._waits(eng, reads, writes)
        self.cnt[eng] += 1
        ev = (eng, self.cnt[eng])
        self._mark(ev, reads, writes)
        self.ops[eng].append((waits, fn, (eng, 1)))
        return ev

    def group(self, eng, fns, reads=(), writes=()):
        waits = self._waits(eng, reads, writes)
        self.cnt[eng] += 1
        ev = (eng, self.cnt[eng])
        self._mark(ev, reads, writes)
        n = len(fns)
        for i, fn in enumerate(fns):
            self.ops[eng].append((waits if i == 0 else [], fn, (eng, 1) if i == n - 1 else None))
        return ev

    def dma(self, queue, out_ap, in_ap, reads=(), writes=(), nowait=False, **kw):
        waits = [] if nowait else self._waits(queue, reads, writes)
        if writes:
            own = writes[0]
            if own.wsem is None:
                own.wsem = self.new_sem("w")
            key = own.wsem
        else:
            own = reads[0]
            if own.rsem is None:
                own.rsem = self.new_sem("r")
            key = own.rsem
        self.semcnt[key] = self.semcnt.get(key, 0) + 16
        ev = (key, self.semcnt[key])
        for b in writes:
            b.writer = ev
            b.readers = {}
        for b in reads:
            b.readers[key] = ev[1]

        def fn(e, out_ap=out_ap, in_ap=in_ap, kw=kw):
            return e.dma_start(out=out_ap, in_=in_ap, **kw)

        self.ops[queue].append((waits, fn, (key, 16)))
        return ev

    def wait_all(self, eng, bufs):
        need = {}
        for b in bufs:
            for ev in [b.writer] + list(b.readers.items()):
                if ev is None:
                    continue
                k, v = ev
                if need.get(k, 0) < v:
                    need[k] = v
        waits = []
        for k, v in need.items():
            if self.seen[eng].get(k, 0) < v:
                self.seen[eng][k] = v
                waits.append((k, v))
        self.ops[eng].append((waits, None, None))

    def barrier(self):
        tgt = {e: self.cnt[e] for e in ENGS if self.cnt[e] > 0}
        for k, v in self.semcnt.items():
            tgt[k] = v
        for e in ENGS:
            waits = []
            for k, v in tgt.items():
                if k == e:
                    continue
                if self.seen[e].get(k, 0) < v:
                    self.seen[e][k] = v
                    waits.append((k, v))
            if self.cnt[e] > 0 and self.seen[e].get(e, 0) < self.cnt[e]:
                self.seen[e][e] = self.cnt[e]
                waits.append((e, self.cnt[e]))
            self.ops[e].append((waits, None, None))

    def _emit(self, e, name):
        for waits, fn, inc in self.ops[name]:
            for k, v in waits:
                e.wait_ge(self.sems[k], v)
            if fn is None:
                continue
            ins = fn(e)
            if inc is None:
                continue
            ins.then_inc(self.sems[inc[0]], inc[1])

    def replay(self):
        with self.nc.Block() as block:
            @block.tensor
            def _(e):
                self._emit(e, "pe")

            @block.scalar
            def _(e):
                self._emit(e, "act")

            @block.vector
            def _(e):
                self._emit(e, "dve")

            @block.gpsimd
            def _(e):
                self._emit(e, "pool")

            @block.sync
            def _(e):
                self._emit(e, "sp")

    def close(self):
        while self._stack:
            self._stack.pop().__exit__(None, None, None)


class Prog:
    def __init__(self, stop_after=None, taps=()):
        self.stop_after = stop_after
        self.taps = list(taps)
        self.nc = bass.Bass("TRN2", target_bir_lowering=False)
        nc = self.nc
        self.S = Sched(nc)
        self._cms = []
        d = {}

        def din(name, shape):
            d[name] = nc.dram_tensor(name, list(shape), F32, kind="ExternalInput").ap()

        din("x", [T, D]); din("p", [DEPTH, T, PLE]); din("consts", [128, NCONST])
        din("attn_norm", [DEPTH, D]); din("w_in", [DEPTH, D, IN_W]); din("m_conv_w", [DEPTH, 4, D])
        din("m_conv_b", [DEPTH, D]); din("m_gate_bias", [DEPTH, 8]); din("m_head_norm", [DEPTH, 512])
        din("a_q_norm", [DEPTH, 64]); din("a_k_norm", [DEPTH, 64]); din("a_lambda", [DEPTH, 4, 64])
        din("a_head_norm", [DEPTH, 512]); din("pool_w", [DEPTH, 4, 128, 128]); din("pool_scale", [DEPTH, 512])
        din("w_branch", [DEPTH, 3, 512, D]); din("w_out", [DEPTH, D, D]); din("ffn_norm", [DEPTH, D])
        din("dense_w_gu", [1, D, 2 * DFF]); din("dense_w_down", [1, DFF, D]); din("router_w", [1, D, NE])
        din("router_b", [1, NE]); din("moe_w_gu", [1, NE, D, 2 * DFE]); din("moe_w_down", [1, NE, DFE, D])
        din("ple_norm", [DEPTH, D]); din("ple_w_gate", [DEPTH, D, D]); din("ple_w_proj", [DEPTH, PLE, D])
        self.d = d
        self.out = nc.dram_tensor("out", [T, D], F32, kind="ExternalOutput").ap()
        self.tap_out = {}

    def sb(self, name, shape, dt):
        cm = self.nc.sbuf_tensor(name, list(shape), dt)
        t = cm.__enter__()
        self._cms.append(cm)
        return t

    def psum(self, name):
        cm = self.nc.psum_tensor(name, [128, 512], F32)
        t = cm.__enter__()
        self._cms.append(cm)
        return t

    def act(self, out, in_, func, reads, writes, bias=0.0, scale=1.0):
        self.S.op("act", lambda e: e.activation(out=out, in_=in_, func=func, bias=bias, scale=scale), reads, writes)

    def tt(self, eng, out, in0, in1, op, reads, writes):
        self.S.op(eng, lambda e: e.tensor_tensor(out, in0, in1, op), reads, writes)

    def ts(self, eng, out, in0, s1, s2, op0, op1, reads, writes):
        if s2 is None:
            self.S.op(eng, lambda e: e.tensor_scalar(out, in0, s1, None, op0), reads, writes)
        else:
            self.S.op(eng, lambda e: e.tensor_scalar(out, in0, s1, s2, op0, op1), reads, writes)

    def stt(self, eng, out, in0, scalar, in1, op0, op1, reads, writes):
        self.S.op(eng, lambda e: e.scalar_tensor_tensor(out, in0, scalar, in1, op0, op1), reads, writes)

    def copy(self, eng, out, in_, reads, writes):
        if eng == "act":
            self.S.op("act", lambda e: e.activation(out=out, in_=in_, func=AF.Copy), reads, writes)
        else:
            self.S.op(eng, lambda e: e.tensor_copy(out, in_), reads, writes)

    def mm(self, out, pairs, reads, writes, start=True, stop=True):
        n = len(pairs)
        fns = []
        for i, (l, r) in enumerate(pairs):
            fns.append(lambda e, l=l, r=r, i=i: e.matmul(out, l, r, start=(start and i == 0), stop=(stop and i == n - 1)))
        self.S.group("pe", fns, reads, writes)

    def mm1(self, out, l, r, reads, writes, start=True, stop=True):
        self.S.op("pe", lambda e: e.matmul(out, l, r, start=start, stop=stop, skip_group_check=True), reads, writes)

    def transpose(self, out, in_, ident, reads, writes):
        self.S.op("pe", lambda e: e.transpose(out, in_, ident), reads, writes)

    def tap(self, name, ap, shape, dt, bufs):
        if name not in self.taps:
            return
        o = self.nc.dram_tensor("tap_" + name, list(shape), dt, kind="ExternalOutput").ap()
        self.tap_out[name] = o
        b = Buf("tap")
        self.S.dma("sp", o, ap, reads=list(bufs))
        self.tapbufs = getattr(self, "tapbufs", []) + list(bufs)

    def wload(self, dram_ap, view):
        i = self.wi % len(self.wbufs)
        self.wi += 1
        t, b = self.wbufs[i]
        dst = view(t)
        self.S.dma("pool", dst, dram_ap, writes=[b])
        return dst, b

    def wload_g(self, dram_fn, ng, view):
        i = self.wi % len(self.wbufs)
        self.wi += 1
        t, b = self.wbufs[i]
        dst = view(t)
        for g in range(ng):
            self.S.dma("pool", dst[:, :, g, :], dram_fn(g), writes=[b])
        return dst, b

    def build(self):
        nc, S, d = self.nc, self.S, self.d
        self.xT = self.sb("xT", [128, KC, T], F32)
        self.hT = self.sb("hT", [128, KC, T], BF16)
        self.xTb = [[Buf("x%d_%d" % (c, tb)) for tb in range(NTB)] for c in range(KC)]
        self.hTb = [[Buf("h%d_%d" % (c, tb)) for tb in range(NTB)] for c in range(KC)]
        self.cf = self.sb("cf", [128, NCONST], F32)
        self.cb = self.sb("cbf", [128, 512], BF16)
        self.cfb, self.cbb = Buf("cf"), Buf("cb")
        self.par = self.sb("par", [128, DEPTH, 128], F32)
        self.parb = Buf("par")
        self.wbuf_bufs = [Buf("wb%d" % i) for i in range(6)]
        self.wbufs = []
        self.wi = 0
        self.ps = [(self.psum("ps%d" % i), Buf("ps%d" % i)) for i in range(8)]
        self.psi = 0
        self.lamt = [self.sb("lamt%d" % l, [128, 4, 64], F32) for l in range(DEPTH)]
        self.lampr = [self.sb("lampr%d" % l, [128, 2, 64], F32) for l in range(DEPTH)]
        rem = nc.sbuf_bytes_remaining
        self.arena_elems = (rem - 2048) // 2
        self.arena = self.sb("arena", [128, self.arena_elems], BF16)
        self.aoff = 0

        cf = self.cf
        self.ident_f = cf[:, CI:CI + 128]
        self.U_f = cf[:, CU:CU + 128]
        self.ones_f = cf[:, CO:CO + 128]
        S.dma("sp", cf[:], d["consts"], writes=[self.cfb])
        self.copy("dve", self.cb[:], cf[:, 0:512], [self.cfb], [self.cbb])
        self.ident_b = self.cb[:, CI:CI + 128]
        self.U_b = self.cb[:, CU:CU + 128]
        self.ones_b = self.cb[:, CO:CO + 128]
        self.blk_b = self.cb[:, CB:CB + 128]

        self.epsg = self.sb("epsg", [128, 1], F32)
        S.op("dve", lambda e: e.memset(self.epsg[:], EPS), [], [self.cfb])
        self.load_params()
        self.load_x()
        for l in range(DEPTH):
            self.layer(l)
            if self.stop_after is not None and self.stop_after[0] == l:
                break
        self.store_x()
        S.replay()
        S.close()
        return nc

    def areset(self):
        self.S.barrier()
        self.aoff = 0

    def set_wbufs(self, n):
        self.wbufs = [(self.aalloc([128, 4096], BF16), self.wbuf_bufs[i]) for i in range(n)]
        self.wi = 0

    def aalloc(self, shape, dt):
        n = int(np.prod(shape[1:]))
        if dt == F32:
            if self.aoff % 2:
                self.aoff += 1
            ne = 2 * n
        else:
            ne = n
        assert self.aoff + ne <= self.arena_elems, ("arena overflow", self.aoff, ne, self.arena_elems)
        v = self.arena[:, self.aoff:self.aoff + ne]
        self.aoff += ne
        if dt == F32:
            v = v.bitcast(F32)
        if len(shape) == 3:
            v = v.rearrange("p (a b) -> p a b", a=shape[1])
        elif len(shape) == 4:
            v = v.rearrange("p (a b c) -> p a b c", a=shape[1], b=shape[2])
        return v

    def nps(self, pool=None):
        pool = pool or (0, 1, 2, 3)
        k = getattr(self, "_psk", {})
        i = k.get(pool, 0)
        k[pool] = i + 1
        self._psk = k
        return self.ps[pool[i % len(pool)]]

    PA_NORM = 0
    PA_CW = 24
    PA_CB = 56
    PA_MHN = 64
    PA_AHN = 68
    PA_PSC = 72
    PA_AQ = 76
    PA_AK = 77
    PA_GB = 78
    PA_RB = 86
    PA_LAM = 94
    PA_TMP = 96

    def load_params(self):
        S, d, par, pb = self.S, self.d, self.par, self.parb
        kw = dict(allow_slow_non_contiguous=True, nowait=True)
        for l in range(DEPTH):
            for i, nm in enumerate(("attn_norm", "ffn_norm", "ple_norm")):
                S.dma("act", par[:, l, self.PA_NORM + 8 * i:self.PA_NORM + 8 * i + 8],
                      d[nm][l].rearrange("(c p) -> p c", p=128), writes=[pb], **kw)
            for j in range(4):
                S.dma("act", par[:, l, self.PA_CW + 8 * j:self.PA_CW + 8 * j + 8],
                      d["m_conv_w"][l, j].rearrange("(c p) -> p c", p=128), writes=[pb], **kw)
            S.dma("act", par[:, l, self.PA_CB:self.PA_CB + 8], d["m_conv_b"][l].rearrange("(c p) -> p c", p=128), writes=[pb], **kw)
            S.dma("act", par[:, l, self.PA_MHN:self.PA_MHN + 4], d["m_head_norm"][l].rearrange("(c p) -> p c", p=128), writes=[pb], **kw)
            S.dma("act", par[:, l, self.PA_AHN:self.PA_AHN + 4], d["a_head_norm"][l].rearrange("(c p) -> p c", p=128), writes=[pb], **kw)
            S.dma("act", par[:, l, self.PA_PSC:self.PA_PSC + 4], d["pool_scale"][l].rearrange("(c p) -> p c", p=128), writes=[pb], **kw)
            for half in range(2):
                S.dma("act", par[half * 64:(half + 1) * 64, l, self.PA_AQ:self.PA_AQ + 1],
                      d["a_q_norm"][l].rearrange("(p o) -> p o", o=1), writes=[pb], **kw)
                S.dma("act", par[half * 64:(half + 1) * 64, l, self.PA_AK:self.PA_AK + 1],
                      d["a_k_norm"][l].rearrange("(p o) -> p o", o=1), writes=[pb], **kw)
            S.dma("act", par[:, l, self.PA_GB:self.PA_GB + 8], d["m_gate_bias"][l].partition_broadcast(128), writes=[pb], **kw)
            if l % 2 == 1:
                S.dma("act", par[:, l, self.PA_RB:self.PA_RB + 8], d["router_b"][l // 2].partition_broadcast(128), writes=[pb], **kw)
            lt = self.lamt[l]
            S.dma("act", lt[:], d["a_lambda"][l].partition_broadcast(128), writes=[pb], **kw)
            tmp = par[:, l, self.PA_TMP:self.PA_TMP + 32]
            pr = self.lampr[l]
            self.tt("dve", pr[:, 0, :], lt[:, 0, :], lt[:, 1, :], ALU.mult, [pb], [pb])
            self.tt("dve", pr[:, 1, :], lt[:, 2, :], lt[:, 3, :], ALU.mult, [pb], [pb])
            S.op("dve", lambda e, pr=pr, tmp=tmp: e.reduce_sum(tmp[:, 0:2], pr[:], AX.X), [pb], [pb])
            self.act(tmp[:, 2:4], tmp[:, 0:2], AF.Exp, [pb], [pb])
            lam_init = 0.8 - 0.6 * math.exp(-0.3 * l)
            self.stt("dve", par[:, l, self.PA_LAM:self.PA_LAM + 1], tmp[:, 3:4], -lam_init, tmp[:, 2:3],
                     ALU.add, ALU.subtract, [pb], [pb])
            self.ts("dve", par[:, l, self.PA_AQ:self.PA_AQ + 1], par[:, l, self.PA_AQ:self.PA_AQ + 1], 0.125, None,
                    ALU.mult, None, [pb], [pb])

    def pcol(self, l, off, n=1):
        return self.par[:, l, off:off + n]

    def load_x(self):
        S, d = self.S, self.d
        self.areset()
        xs = [self.aalloc([128, D], F32) for _ in range(2)]
        xsb = [Buf("xs0"), Buf("xs1")]
        for tt in range(NTT):
            s, sbuf = xs[tt % 2], xsb[tt % 2]
            S.dma("sp", s, d["x"][tt * 128:(tt + 1) * 128, :], writes=[sbuf])
            for half in range(2):
                pt, pb = self.nps()
                for i in range(4):
                    c = half * 4 + i
                    self.transpose(pt[:, i * 128:(i + 1) * 128], s[:, c * 128:(c + 1) * 128], self.ident_f,
                                   [sbuf, self.cfb], [pb])
                tb = tt // 4
                self.copy("dve" if half == 0 else "act",
                          self.xT[:, half * 4:half * 4 + 4, tt * 128:(tt + 1) * 128],
                          pt[:].rearrange("p (a b) -> p a b", a=4), [pb],
                          [self.xTb[half * 4 + i][tb] for i in range(4)])

    def store_x(self):
        S = self.S
        self.areset()
        xs = [self.aalloc([128, D], F32) for _ in range(2)]
        xsb = [Buf("os0"), Buf("os1")]
        for tt in range(NTT):
            s, sbuf = xs[tt % 2], xsb[tt % 2]
            tb = tt // 4
            for half in range(2):
                pt, pb = self.nps()
                for i in range(4):
                    c = half * 4 + i
                    self.transpose(pt[:, i * 128:(i + 1) * 128], self.xT[:, c, tt * 128:(tt + 1) * 128], self.ident_f,
                                   [self.xTb[c][tb], self.cfb], [pb])
                self.copy("dve" if half == 0 else "act", s[:, half * 512:(half + 1) * 512], pt[:], [pb], [sbuf])
            S.dma("sp", self.out[tt * 128:(tt + 1) * 128, :], s, reads=[sbuf])
        S.wait_all("sp", xsb + getattr(self, "tapbufs", []))

    def rmsnorm(self, l, which):
        gcol = self.PA_NORM + 8 * which
        pend = {}

        def emit_sq(tb):
            sl = slice(tb * TB, (tb + 1) * TB)
            sq, sqb = self.n_sq[tb % 2], self.n_sqb[tb % 2]
            for c in range(KC):
                if c < 6:
                    self.act(sq[:, c, :], self.xT[:, c, sl], AF.Square, [self.xTb[c][tb]], [sqb[c]])
                else:
                    self.tt("dve", sq[:, c, :], self.xT[:, c, sl], self.xT[:, c, sl], ALU.mult, [self.xTb[c][tb]], [sqb[c]])
            pt, pb = self.nps((6, 7))
            self.mm(pt[:], [(self.ones_b, sq[:, c, :]) for c in range(KC)], sqb + [self.cbb], [pb])
            pend[tb] = (pt, pb)

        def emit_fin(tb):
            sl = slice(tb * TB, (tb + 1) * TB)
            rs, rsb = self.n_rs[tb % 2], self.n_rsb[tb % 2]
            pt, pb = pend.pop(tb)
            self.act(rs[:], pt[:], AF.Ln, [pb, self.cfb], [rsb], bias=self.epsg[:, 0:1], scale=1.0 / D)
            self.act(rs[:], rs[:], AF.Exp, [rsb], [rsb], scale=-0.5)
            for c in range(KC):
                self.stt("dve", self.hT[:, c, sl], self.xT[:, c, sl],
                         self.pcol(l, gcol + c), rs[:], ALU.mult, ALU.mult,
                         [self.xTb[c][tb], rsb, self.parb], [self.hTb[c][tb]])

        emit_sq(0)
        for tb in range(NTB):
            if tb + 1 < NTB:
                emit_sq(tb + 1)
            emit_fin(tb)

    def alloc_norm_tmp(self):
        self.n_sq = [self.aalloc([128, KC, TB], BF16) for _ in range(2)]
        self.n_sqb = [[Buf("sq%d_%d" % (i, c)) for c in range(KC)] for i in range(2)]
        self.n_rs = [self.aalloc([128, TB], F32) for _ in range(2)]
        self.n_rsb = [Buf("rs0"), Buf("rs1")]

    def hreads(self, tb):
        return [self.hTb[c][tb] for c in range(KC)]

    def layer(self, l):
        S = self.S
        st = self.stop_after
        self.areset()
        self.alloc_norm_tmp()
        self.rmsnorm(l, 0)
        self.tap("h%d" % l, self.hT[:], [128, KC, T], BF16, [b for r in self.hTb for b in r])
        if st == (l, "norm1"):
            return
        self.areset()
        self.set_wbufs(2)
        self.zT = self.aalloc([128, KC, T], BF16)
        self.zTb = [[Buf("z%d_%d" % (c, tb)) for tb in range(NTB)] for c in range(KC)]
        self.hb = self.aalloc([128, 4, T], BF16)
        self.hbb = [[Buf("hb%d_%d" % (c, tb)) for tb in range(NTB)] for c in range(4)]
        base2 = self.aoff
        for r, fn in enumerate((self.mlstm, self.dattn, self.poolb)):
            S.barrier()
            self.aoff = base2
            fn(l)
            self.tap(("hm%d", "ha%d", "hp%d")[r] % l, self.hb[:], [128, 4, T], BF16, [b for rr in self.hbb for b in rr])
            if st == (l, ("hm", "ha", "hp")[r]):
                return
            S.barrier()
            self.aoff = base2
            self.merge(l, r)
        self.tap("z%d" % l, self.zT[:], [128, KC, T], BF16, [b for r in self.zTb for b in r])
        self.wout(l)
        if st == (l, "mix"):
            return
        self.areset()
        self.alloc_norm_tmp()
        self.rmsnorm(l, 1)
        self.set_wbufs(6)
        self.alloc_ffn_tmp()
        if l % 2 == 0:
            self.ffn(l, None)
        else:
            self.moe(l)
        if st == (l, "ffn"):
            return
        self.areset()
        self.alloc_norm_tmp()
        self.rmsnorm(l, 2)
        self.set_wbufs(3)
        self.ple(l)

    def proj_fm(self, w, wb, m, rhs_fn, nk, pool=None):
        for tb in range(NTB):
            pt, pb = self.nps(pool)
            pairs, reads = [], [wb]
            for kc in range(nk):
                r, rb = rhs_fn(kc, tb)
                pairs.append((w[:, kc, m * 128:(m + 1) * 128], r))
                reads.append(rb)
            self.mm(pt[:], pairs, reads, [pb])
            yield tb, pt, pb

    def h_rhs(self, kc, tb):
        return self.hT[:, kc, tb * TB:(tb + 1) * TB], self.hTb[kc][tb]

    def proj_tm(self, w_fn, wb, out_v, out_vb, nk=KC):
        for q4 in range(4):
            pt, pb = self.nps()
            for i in range(4):
                tt = q4 * 4 + i
                tb = tt // 4
                pairs = [(self.hT[:, kc, tt * 128:(tt + 1) * 128], w_fn(kc)) for kc in range(nk)]
                self.mm(pt[:, i * 128:(i + 1) * 128], pairs, self.hreads(tb) + [wb], [pb])
            self.copy("act", out_v[:, q4 * 4:q4 * 4 + 4, :], pt[:].rearrange("p (a b) -> p a b", a=4), [pb], [out_vb])

    def mlstm(self, l):
        S, d = self.S, self.d
        w_in = d["w_in"][l]
        wg, wgb = self.wload(w_in[:, C_MI:C_MI + 8].rearrange("(c p) n -> p c n", p=128),
                             lambda t: t[:, 0:64].rearrange("p (c n) -> p c n", c=KC))
        GT = self.aalloc([128, NTT, 8], F32)
        nlf = self.aalloc([128, NTT, 4], F32)
        acol = self.aalloc([128, NTT, 4], F32)
        gb = Buf("gates")
        pt, pb = self.nps((6, 7))
        for tt in range(NTT):
            pairs = [(self.hT[:, kc, tt * 128:(tt + 1) * 128], wg[:, kc, :]) for kc in range(KC)]
            self.mm(pt[:, tt * 8:(tt + 1) * 8], pairs, self.hreads(tt // 4) + [wgb], [pb])
        self.tt("dve", GT[:], pt[:, 0:128].rearrange("p (a b) -> p a b", a=NTT),
                self.pcol(l, self.PA_GB, 8).unsqueeze(1).broadcast_to([128, NTT, 8]), ALU.add, [pb, self.parb], [gb])
        self.act(nlf[:], GT[:, :, 4:8], AF.Exp, [gb], [gb], scale=-1.0)
        self.act(nlf[:], nlf[:], AF.Ln, [gb], [gb], bias=1.0)
        pt, pb = self.nps((6, 7))
        self.mm1(pt[:, 0:64], self.U_f, nlf[:].rearrange("p a b -> p (a b)"), [gb, self.cfb], [pb])
        self.stt("dve", acol[:], pt[:, 0:64].rearrange("p (a b) -> p a b", a=NTT), LNS, GT[:, :, 0:4],
                 ALU.add, ALU.add, [pb, gb], [gb])

        pre1 = self.aalloc([128, T + 4], BF16)
        pre = [pre1, pre1]
        preb1 = Buf("pre")
        preb = [preb1, preb1]
        dgt = self.aalloc([128, 8, 128], BF16)
        dgtb = Buf("dgt")
        qk = [self.aalloc([128, T], BF16) for _ in range(2)]
        qkb = [Buf("q"), Buf("k")]
        vh = self.aalloc([128, NTT, 128], BF16)
        vhb = Buf("vh")
        og = self.aalloc([128, T], BF16)
        ogb = [Buf("og%d" % tb) for tb in range(NTB)]
        NU = self.aalloc([128, 128], BF16); NUb = Buf("NU")
        self.ts("dve", NU[:], self.U_f, -30000.0, 30000.0, ALU.mult, ALU.add, [self.cfb], [NUb])
        rhsb_t = [self.aalloc([128, 128], F32) for _ in range(3)]; rhsbb = [Buf("rhsb%d" % i) for i in range(3)]
        DT = [self.aalloc([128, 128], F32) for _ in range(2)]; DTb = [Buf("DT0"), Buf("DT1")]
        ebt = [self.aalloc([128, 128], F32) for _ in range(3)]; ebb = [Buf("eb%d" % i) for i in range(3)]
        epsc = self.aalloc([128, 2], F32)
        S.op("dve", lambda e: e.memset(epsc[:, 0:1], EPS), [], [self.parb])
        ekl = [self.aalloc([128, 2], F32) for _ in range(2)]; eklb = [Buf("ekl0"), Buf("ekl1")]
        STm = [self.aalloc([128, 128], BF16) for _ in range(2)]; STmb = [Buf("STm0"), Buf("STm1")]
        K2 = [self.aalloc([128, 128], BF16) for _ in range(2)]; K2b = [Buf("K20"), Buf("K21")]
        qp = [self.aalloc([128, 128], BF16) for _ in range(2)]; qpb = [Buf("qp0"), Buf("qp1")]
        Cn = self.aalloc([128, 256], F32); Cnb = Buf("Cn")
        Cbf = self.aalloc([128, 256], BF16); Cbfb = Buf("Cbf")
        tmp = self.aalloc([128, 128], F32); tmpb = Buf("tmp")
        hraw = [self.aalloc([128, TB], BF16) for _ in range(2)]; hrawb = [Buf("hraw0"), Buf("hraw1")]
        sqh = self.aalloc([128, TB], BF16); sqhb = Buf("sqh")
        rsh = self.aalloc([128, TB], F32); rshb = Buf("rsh")
        S.op("dve", lambda e: e.memset(pre1[:, 0:4], 0.0), [], [preb1])

        def load_w4(h):
            return self.wload_g(
                lambda g, h=h: w_in[:, g * 512 + h * 128:g * 512 + (h + 1) * 128].rearrange("(c p) n -> p c n", p=128), 4,
                lambda t: t[:].rearrange("p (c g n) -> p c g n", c=KC, g=4))

        w4n = load_w4(0)
        for h in range(4):
            w4, w4b = w4n
            self.proj_tm(lambda kc: w4[:, kc, 2, :], w4b, vh, vhb)
            for tb in range(NTB):
                pt, pb = self.nps()
                self.mm(pt[:], [(w4[:, kc, 3, :], self.hT[:, kc, tb * TB:(tb + 1) * TB]) for kc in range(KC)],
                        self.hreads(tb) + [w4b], [pb])
                self.act(og[:, tb * TB:(tb + 1) * TB], pt[:], AF.Sigmoid, [pb], [ogb[tb]])
            for j in range(2):
                ch = j * 4 + h
                for tap in range(4):
                    self.ts("dve", dgt[:, j * 4 + tap, :], self.ident_f, self.pcol(l, self.PA_CW + 8 * tap + ch), None,
                            ALU.mult, None, [self.cfb, self.parb], [dgtb])
            for j in range(2):
                ch = j * 4 + h
                for tb in range(NTB):
                    pt, pb = self.nps()
                    self.mm(pt[:], [(w4[:, kc, j, :], self.hT[:, kc, tb * TB:(tb + 1) * TB]) for kc in range(KC)],
                            self.hreads(tb) + [w4b], [pb])
                    self.copy("act" if tb % 2 == 0 else "dve", pre1[:, 3 + tb * TB:3 + (tb + 1) * TB], pt[:], [pb], [preb1])
                for tb in range(NTB):
                    pt, pb = self.nps()
                    self.mm(pt[:], [(dgt[:, j * 4 + tap, :], pre1[:, tb * TB + tap:tb * TB + tap + TB]) for tap in range(4)],
                            [dgtb, preb1], [pb])
                    self.act(qk[j][:, tb * TB:(tb + 1) * TB], pt[:], AF.Silu, [pb, self.parb], [qkb[j]],
                             bias=self.pcol(l, self.PA_CB + ch))
            q, k = qk
            if h + 1 < 4:
                w4n = load_w4(h + 1)
            stA, stB = {}, {}
            norm_pending = []

            def stage_a(c):
                cs = slice(c * 128, (c + 1) * 128)
                r_, rb_ = rhsb_t[c % 3], rhsbb[c % 3]
                self.ts("pool", r_[:], self.U_f, nlf[:, c, h:h + 1], None, ALU.mult, None, [self.cfb, gb], [rb_])
                nb, nbb = self.nps((6, 7))
                self.mm1(nb[:, 0:128], self.ones_f, r_[:], [rb_, self.cfb], [nbb])
                self.mm(nb[:, 128:256], [(self.ones_f, r_[:]), (self.ident_b, NU[:])], [rb_, self.cfb, self.cbb, NUb], [nbb])
                st, stb = self.nps((0, 1))
                self.mm1(st[:, 0:128], k[:, cs], q[:, cs], [qkb[0], qkb[1]], [stb])
                trv = None
                if c < NTT - 1:
                    trv = st[:].bitcast(BF16)[:, 512:640]
                    self.transpose(trv, k[:, cs], self.ident_b, [qkb[1], self.cbb], [stb])
                stA[c] = (nb, nbb, st, stb, trv)

            def stage_b(c):
                cs = slice(c * 128, (c + 1) * 128)
                nb, nbb, st, stb, trv = stA.pop(c)
                i2, i3 = c % 2, c % 3
                self.act(DT[i2][:], nb[:, 128:256], AF.Exp, [nbb, gb], [DTb[i2]], bias=acol[:, c, h:h + 1], scale=-1.0)
                self.act(ebt[i3][:], nb[:, 0:128], AF.Exp, [nbb], [ebb[i3]], scale=-1.0)
                self.tt("dve", STm[i2][:], st[:, 0:128], DT[i2][:], ALU.mult, [stb, DTb[i2]], [STmb[i2]])
                if c < NTT - 1:
                    self.act(ekl[i2][:, 0:1], nb[:, 127:128], AF.Exp, [nbb, gb], [eklb[i2]], bias=acol[:, c, h:h + 1],
                             scale=-1.0)
                    self.ts("dve", K2[i2][:], trv, ekl[i2][:, 0:1], None, ALU.mult, None, [stb, eklb[i2]], [K2b[i2]])
                if c > 0:
                    self.tt("pool", qp[i2][:], q[:, cs], ebt[i3][:], ALU.mult, [qkb[0], ebb[i3]], [qpb[i2]])

            def stage_c(c):
                cs = slice(c * 128, (c + 1) * 128)
                i2, i3 = c % 2, c % 3
                if c < NTT - 1:
                    kv, kvb = self.ps[2]
                    self.mm1(kv[:, 0:128], K2[i2][:], vh[:, c, :], [K2b[i2], vhb], [kvb])
                    self.mm1(kv[:, 128:256], K2[i2][:], self.ones_b, [K2b[i2], self.cbb], [kvb])
                nd, ndb = self.nps((4, 5))
                last = (c == 0)
                self.mm1(nd[:, 0:128], vh[:, c, :], STm[i2][:], [vhb, STmb[i2]], [ndb], start=True, stop=last)
                if c > 0:
                    self.mm1(nd[:, 0:128], Cbf[:, 0:128], qp[i2][:], [Cbfb, qpb[i2]], [ndb], start=False, stop=True)
                self.mm1(nd[:, 128:256], self.ones_b, STm[i2][:], [self.cbb, STmb[i2]], [ndb], start=True, stop=last)
                if c > 0:
                    self.mm1(nd[:, 128:256], Cbf[:, 128:256], qp[i2][:], [Cbfb, qpb[i2]], [ndb], start=False, stop=True)
                if c < NTT - 1:
                    if c == 0:
                        self.copy("dve", Cn[:], kv[:, 0:256], [kvb], [Cnb])
                    else:
                        self.stt("dve", Cn[:], Cn[:], ebt[i3][:, 127:128], kv[:, 0:256], ALU.mult, ALU.add,
                                 [Cnb, ebb[i3], kvb], [Cnb])
                    self.copy("act", Cbf[:], Cn[:], [Cnb], [Cbfb])
                self.ts("dve", tmp[:], nd[:, 128:256], -1.0, 1.0, ALU.mult, ALU.max, [ndb], [tmpb])
                self.tt("dve", tmp[:], tmp[:], nd[:, 128:256], ALU.max, [tmpb, ndb], [tmpb])
                S.op("dve", lambda e: e.reciprocal(tmp[:], tmp[:]), [tmpb], [tmpb])
                ci = c % 4
                hi = (c // 4) % 2
                self.tt("dve", hraw[hi][:, ci * 128:(ci + 1) * 128], nd[:, 0:128], tmp[:], ALU.mult, [ndb, tmpb], [hrawb[hi]])
                if ci == 3:
                    self.tt("pool", sqh[:], hraw[hi][:], hraw[hi][:], ALU.mult, [hrawb[hi]], [sqhb])
                    norm_pending.append(c)

            def norm_tail(c):
                tb = c // 4
                hi = tb % 2
                ss, ssb = self.ps[3]
                self.mm1(ss[:], self.ones_b, sqh[:], [self.cbb, sqhb], [ssb])
                self.act(rsh[:], ss[:], AF.Ln, [ssb, self.parb], [rshb], bias=epsc[:, 0:1], scale=1.0 / 128)
                self.act(rsh[:], rsh[:], AF.Exp, [rshb], [rshb], scale=-0.5)
                self.tt("dve", rsh[:], hraw[hi][:], rsh[:], ALU.mult, [hrawb[hi], rshb], [rshb])
                self.stt("dve", self.hb[:, h, tb * TB:(tb + 1) * TB], rsh[:], self.pcol(l, self.PA_MHN + h),
                         og[:, tb * TB:(tb + 1) * TB], ALU.mult, ALU.mult, [rshb, self.parb, ogb[tb]],
                         [self.hbb[h][tb]])

            stage_a(0)
            stage_a(1)
            stage_b(0)
            for c in range(NTT):
                if c + 2 < NTT:
                    stage_a(c + 2)
                if c + 1 < NTT:
                    stage_b(c + 1)
                if norm_pending:
                    norm_tail(norm_pending.pop(0))
                stage_c(c)
            while norm_pending:
                norm_tail(norm_pending.pop(0))

    def dattn(self, l):
        S, d = self.S, self.d
        w_in = d["w_in"][l]
        lam_init = 0.8 - 0.6 * math.exp(-0.3 * l)
        qn = self.aalloc([128, T], BF16); qnb = [Buf("qn%d" % tb) for tb in range(NTB)]
        knt = self.aalloc([128, TB], BF16); kntb = Buf("knt")
        knz = [self.aalloc([128, T], BF16) for _ in range(2)]
        knb = [Buf("kn%d" % tb) for tb in range(NTB)]
        kn = None
        vh = self.aalloc([128, NTT, 128], BF16); vhb = Buf("vha")
        raw = [self.aalloc([128, TB], F32) for _ in range(2)]; rawb = [Buf("raw0"), Buf("raw1")]
        sq = [self.aalloc([128, TB], BF16) for _ in range(2)]; sqb = [Buf("sqa0"), Buf("sqa1")]
        rs = [self.aalloc([128, TB], F32) for _ in range(2)]; rsb = [Buf("rsa0"), Buf("rsa1")]
        NEG = self.aalloc([128, 128], BF16); NEGb = Buf("NEG")
        self.ts("dve", NEG[:], self.U_f, 30000.0, -30000.0, ALU.mult, ALU.add, [self.cfb], [NEGb])
        P = [self.aalloc([128, 2, QB], BF16) for _ in range(3)]
        Pb = [Buf("P0"), Buf("P1"), Buf("P2")]
        self.epsc = self.aalloc([128, 2], F32)
        S.op("dve", lambda e: e.memset(self.epsc[:, 0:1], EPS), [], [self.parb])
        S.op("dve", lambda e: e.memset(self.epsc[:, 1:2], EPS / (1.0 - lam_init) ** 2), [], [self.parb])
        rden = self.aalloc([128, 2 * QB], F32); rdenb = Buf("rden")
        tq = self.aalloc([128, 2 * QB], F32); tqb = Buf("tq")
        od = [self.aalloc([128, QB], F32) for _ in range(2)]; odb = [Buf("od0"), Buf("od1")]
        sqo = [self.aalloc([128, QB], BF16) for _ in range(2)]; sqob = [Buf("sqo0"), Buf("sqo1")]
        rso = self.aalloc([128, QB], F32); rsob = Buf("rso")
        pi = 0
        def load_w3(h):
            return self.wload_g(
                lambda g, h=h: w_in[:, C_AQ + g * 512 + h * 128:C_AQ + g * 512 + (h + 1) * 128].rearrange(
                    "(c p) n -> p c n", p=128), 3,
                lambda t: t[:, 0:3072].rearrange("p (c g n) -> p c g n", c=KC, g=3))

        w3n = load_w3(0)
        for h in range(4):
            w3, w3b = w3n
            items = [(tb, j) for tb in range(NTB) for j in range(2)]

            def qk_stage1(n):
                tb, j = items[n]
                raw_, rawb_ = raw[n % 2], rawb[n % 2]
                sq_, sqb_ = sq[n % 2], sqb[n % 2]
                pt, pb = self.nps()
                self.mm(pt[:], [(w3[:, kc, j, :], self.hT[:, kc, tb * TB:(tb + 1) * TB]) for kc in range(KC)],
                        self.hreads(tb) + [w3b], [pb])
                self.copy("act", raw_[:], pt[:], [pb], [rawb_])
                self.tt("pool" if n % 2 else "dve", sq_[:], raw_[:], raw_[:], ALU.mult, [rawb_], [sqb_])

            def qk_stage2(n):
                tb, j = items[n]
                raw_, rawb_ = raw[n % 2], rawb[n % 2]
                sq_, sqb_ = sq[n % 2], sqb[n % 2]
                rs_, rsb_ = rs[n % 2], rsb[n % 2]
                gc = self.PA_AQ if j == 0 else self.PA_AK
                ss, ssb = self.nps((6, 7))
                self.mm1(ss[:], self.blk_b, sq_[:], [self.cbb, sqb_], [ssb])
                self.act(rs_[:], ss[:], AF.Ln, [ssb, self.parb], [rsb_], bias=self.epsc[:, 0:1], scale=1.0 / 64)
                self.act(rs_[:], rs_[:], AF.Exp, [rsb_], [rsb_], scale=-0.5)
                if j == 0:
                    self.stt("dve", qn[:, tb * TB:(tb + 1) * TB], raw_[:], self.pcol(l, gc), rs_[:], ALU.mult, ALU.mult,
                             [rawb_, rsb_, self.parb], [qnb[tb]])
                else:
                    self.stt("dve", knt[:], raw_[:], self.pcol(l, gc), rs_[:], ALU.mult, ALU.mult,
                             [rawb_, rsb_, self.parb], [kntb])
                    for m in range(2):
                        self.ts("pool" if m else "dve", knz[m][:, tb * TB:(tb + 1) * TB], knt[:],
                                self.cf[:, CB + 64 * m:CB + 64 * m + 1], None, ALU.mult, None,
                                [kntb, self.cfb], [knb[tb]])

            qk_stage1(0)
            for n in range(len(items)):
                if n + 1 < len(items):
                    qk_stage1(n + 1)
                qk_stage2(n)
            self.proj_tm(lambda kc: w3[:, kc, 2, :], w3b, vh, vhb)
            blocks = []
            for qb in range(T // QB):
                nkt = 2 * qb + 2
                for kt in range(nkt):
                    blocks.append((qb, kt, nkt))
            accp = [(self.ps[3], self.ps[4]), (self.ps[5], self.ps[6])]
            state = {}

            def emit_S(i):
                qb, kt, nkt = blocks[i]
                q0, k0 = qb * QB, kt * 128
                lo = 128 if kt == nkt - 1 else 0
                diag = kt >= nkt - 2
                sp_, spb = self.nps((0, 1, 2))
                for m in range(2):
                    self.mm1(sp_[:, m * QB + lo:(m + 1) * QB], knz[m][:, k0:k0 + 128],
                             qn[:, q0 + lo:q0 + QB], [knb[k0 // TB], qnb[q0 // TB]], [spb], start=True, stop=not diag)
                    if diag:
                        self.mm1(sp_[:, m * QB + lo:m * QB + lo + 128], self.ident_b, NEG[:], [self.cbb, NEGb], [spb],
                                 start=False, stop=True)
                state[i] = (sp_, spb)

            def emit_rest(i):
                qb, kt, nkt = blocks[i]
                q0, k0 = qb * QB, kt * 128
                tbq = q0 // TB
                lo = 128 if kt == nkt - 1 else 0
                diag = kt >= nkt - 2
                di = (q0 - k0) // 128 + 1
                sp_, spb = state.pop(i)
                (ops, opb), (dps, dpb) = accp[qb % 2]
                Pt, Ptb = P[i % 3], Pb[i % 3]
                spv = sp_[:].rearrange("p (a b) -> p a b", a=2)
                self.act(Pt[:, :, lo:QB], spv[:, :, lo:QB], AF.Exp, [spb, self.cfb], [Ptb],
                         bias=self.cf[:, CAL + h * 16 + di:CAL + h * 16 + di + 1])
                first = kt == 0
                if lo == 0:
                    self.mm1(ops[:], vh[:, kt, :], Pt[:].rearrange("p a b -> p (a b)"), [vhb, Ptb], [opb],
                             start=first, stop=False)
                    self.mm1(dps[:], self.ones_b, Pt[:].rearrange("p a b -> p (a b)"), [self.cbb, Ptb], [dpb],
                             start=first, stop=False)
                else:
                    for m in range(2):
                        self.mm1(ops[:, m * QB + lo:(m + 1) * QB], vh[:, kt, :], Pt[:, m, lo:QB], [vhb, Ptb], [opb],
                                 start=False, stop=True)
                        self.mm1(dps[:, m * QB + lo:(m + 1) * QB], self.ones_b, Pt[:, m, lo:QB], [self.cbb, Ptb], [dpb],
                                 start=False, stop=True)
                if kt == nkt - 1:
                    S.op("dve", lambda e, dps=dps: e.reciprocal(rden[:], dps[:]), [dpb], [rdenb])
                    self.tt("dve", tq[:], ops[:], rden[:], ALU.mult, [opb, rdenb], [tqb])
                    odt, odtb = od[qb % 2], odb[qb % 2]
                    self.stt("dve", odt[:], tq[:, QB:2 * QB], self.pcol(l, self.PA_LAM), tq[:, 0:QB], ALU.mult, ALU.add,
                             [tqb, self.parb], [odtb])
                    self.tt("pool", sqo[qb % 2][:], odt[:], odt[:], ALU.mult, [odtb], [sqob[qb % 2]])
                    pending.append((i + 6, qb))

            def emit_post_b(qb):
                q0 = qb * QB
                tbq = q0 // TB
                odt, odtb = od[qb % 2], odb[qb % 2]
                ss, ssb = self.ps[7]
                self.mm1(ss[:, 0:QB], self.ones_b, sqo[qb % 2][:], [self.cbb, sqob[qb % 2]], [ssb])
                sc = 1.0 / (1.0 - lam_init) ** 2
                self.act(rso[:], ss[:, 0:QB], AF.Ln, [ssb, self.parb], [rsob], bias=self.epsc[:, 1:2], scale=sc / 128)
                self.act(rso[:], rso[:], AF.Exp, [rsob], [rsob], scale=-0.5)
                self.stt("dve", self.hb[:, h, q0:q0 + QB], odt[:], self.pcol(l, self.PA_AHN + h), rso[:], ALU.mult,
                         ALU.mult, [odtb, rsob, self.parb], [self.hbb[h][tbq]])

            pending = []
            if h + 1 < 4:
                w3n = load_w3(h + 1)
            emit_S(0)
            emit_S(1)
            for i in range(len(blocks)):
                if i + 2 < len(blocks):
                    emit_S(i + 2)
                emit_rest(i)
                while pending and pending[0][0] <= i:
                    emit_post_b(pending.pop(0)[1])
            while pending:
                emit_post_b(pending.pop(0)[1])

    def poolb(self, l):
        S, d = self.S, self.d
        w_in = d["w_in"][l]
        PAD = 16
        ub = self.aalloc([128, PAD + T], F32); ubb = Buf("u")
        A = self.aalloc([128, PAD + T], F32); Ab = Buf("A")
        Bt = self.aalloc([128, PAD + T], F32); Bb = Buf("B")
        pl = self.aalloc([128, T], BF16); plb = Buf("pl")
        for t_, b_ in ((ub, ubb), (A, Ab), (Bt, Bb)):
            S.op("pool", lambda e, t_=t_: e.memset(t_[:, 0:PAD], 0.0), [], [b_])
        w, wb = self.wload(w_in[:, C_PU:C_PU + 512].rearrange("(c p) n -> p c n", p=128),
                           lambda t: t[:].rearrange("p (c n) -> p c n", c=KC))
        pw, pwb = self.wload(d["pool_w"][l].rearrange("g c d -> c g d"),
                             lambda t: t[:, 0:512].rearrange("p (g d) -> p g d", g=4))
        for g in range(4):
            for tb, pt, pb in self.proj_fm(w, wb, g, self.h_rhs, KC):
                self.copy("act", ub[:, PAD + tb * TB:PAD + (tb + 1) * TB], pt[:], [pb], [ubb])
            src, srcb = ub, ubb
            bufs = [(A, Ab), (Bt, Bb)]
            for step in range(g + 1):
                sh = 1 << step
                dst, dstb = bufs[step % 2]
                self.tt("dve" if step % 2 == 0 else "pool", dst[:, PAD:PAD + T], src[:, PAD:PAD + T],
                        src[:, PAD - sh:PAD - sh + T], ALU.add, [srcb], [dstb])
                src, srcb = dst, dstb
            wn = PWIN[g]
            self.tt("dve", src[:, PAD:PAD + 16], src[:, PAD:PAD + 16], self.cf[:, CPC + g * 16:CPC + g * 16 + 16],
                    ALU.mult, [srcb, self.cfb], [srcb])
            self.stt("dve", pl[:], src[:, PAD:PAD + T], 1.0 / wn, ub[:, PAD:PAD + T], ALU.mult, ALU.subtract,
                     [srcb, ubb], [plb])
            for tb in range(NTB):
                pt, pb = self.nps()
                self.mm1(pt[:], pw[:, g, :], pl[:, tb * TB:(tb + 1) * TB], [pwb, plb], [pb])
                self.act(self.hb[:, g, tb * TB:(tb + 1) * TB], pt[:], AF.Copy, [pb, self.parb], [self.hbb[g][tb]],
                         scale=self.pcol(l, self.PA_PSC + g))

    def merge(self, l, r):
        d = self.d
        w_in = d["w_in"][l]
        gate = [self.aalloc([128, TB], BF16) for _ in range(2)]
        gateb = [Buf("gate0"), Buf("gate1")]
        prod = [self.aalloc([128, TB], BF16) for _ in range(2)]
        prodb = [Buf("prod0"), Buf("prod1")]
        gi = 0
        for cg in range(2):
            c0 = C_G + r * D + cg * 512
            wg, wgb = self.wload(w_in[:, c0:c0 + 512].rearrange("(c p) n -> p c n", p=128),
                                 lambda t: t[:].rearrange("p (c n) -> p c n", c=KC))
            wbr, wbrb = self.wload(d["w_branch"][l, r][:, cg * 512:(cg + 1) * 512].rearrange("(c p) n -> p c n", p=128),
                                   lambda t: t[:, 0:2048].rearrange("p (c n) -> p c n", c=4))
            for m in range(4):
                c = cg * 4 + m
                for tb in range(NTB):
                    sl = slice(tb * TB, (tb + 1) * TB)
                    g_, gb_ = gate[gi % 2], gateb[gi % 2]
                    p_, pb_ = prod[gi % 2], prodb[gi % 2]
                    gi += 1
                    pt, pb = self.nps()
                    self.mm(pt[:], [(wg[:, kc, m * 128:(m + 1) * 128], self.hT[:, kc, sl]) for kc in range(KC)],
                            self.hreads(tb) + [wgb], [pb])
                    self.act(g_[:], pt[:], AF.Sigmoid, [pb], [gb_])
                    pt2, pb2 = self.nps()
                    self.mm(pt2[:], [(wbr[:, kc, m * 128:(m + 1) * 128], self.hb[:, kc, sl]) for kc in range(4)],
                            [self.hbb[kc][tb] for kc in range(4)] + [wbrb], [pb2])
                    if r == 0:
                        self.tt("dve", self.zT[:, c, sl], pt2[:], g_[:], ALU.mult, [pb2, gb_], [self.zTb[c][tb]])
                    else:
                        self.tt("dve", p_[:], pt2[:], g_[:], ALU.mult, [pb2, gb_], [pb_])
                        self.tt("dve", self.zT[:, c, sl], self.zT[:, c, sl], p_[:], ALU.add,
                                [self.zTb[c][tb], pb_], [self.zTb[c][tb]])

    def wout(self, l):
        d = self.d
        for cg in range(2):
            w, wb = self.wload(d["w_out"][l][:, cg * 512:(cg + 1) * 512].rearrange("(c p) n -> p c n", p=128),
                               lambda t: t[:].rearrange("p (c n) -> p c n", c=KC))
            for m in range(4):
                c = cg * 4 + m
                for tb in range(NTB):
                    sl = slice(tb * TB, (tb + 1) * TB)
                    pt, pb = self.nps()
                    self.mm(pt[:], [(w[:, kc, m * 128:(m + 1) * 128], self.zT[:, kc, sl]) for kc in range(KC)],
                            [self.zTb[kc][tb] for kc in range(KC)] + [wb], [pb])
                    self.tt("dve", self.xT[:, c, sl], pt[:], self.xT[:, c, sl], ALU.add, [pb, self.xTb[c][tb]],
                            [self.xTb[c][tb]])

    def ffn(self, l, expert, cbrow=None, cbrowb=None):
        d = self.d
        if expert is None:
            wgu = d["dense_w_gu"][l // 2]
            wdn = d["dense_w_down"][l // 2]
            dff = DFF
        else:
            wgu = d["moe_w_gu"][l // 2, expert]
            wdn = d["moe_w_down"][l // 2, expert]
            dff = DFE
        nj = dff // 128
        a, ab = self.f_a, self.f_ab
        sg, sgb = self.f_sg, self.f_sgb
        si = 0
        j0 = 0
        while j0 < nj:
            n = min(4, nj - j0)
            wg_, wgb_ = self.wload(wgu[:, j0 * 128:(j0 + n) * 128].rearrange("(c p) n -> p c n", p=128),
                                   lambda t: t[:, 0:KC * n * 128].rearrange("p (c n) -> p c n", c=KC))
            wu_, wub_ = self.wload(wgu[:, dff + j0 * 128:dff + (j0 + n) * 128].rearrange("(c p) n -> p c n", p=128),
                                   lambda t: t[:, 0:KC * n * 128].rearrange("p (c n) -> p c n", c=KC))
            wd_, wdb_ = self.wload(wdn[j0 * 128:(j0 + n) * 128, :].rearrange("(j p) n -> p j n", p=128),
                                   lambda t: t[:, 0:n * D].rearrange("p (j n) -> p j n", j=n))
            for jj in range(n):
                for tb in range(NTB):
                    sl = slice(tb * TB, (tb + 1) * TB)
                    s_, sb_ = sg[si % 2], sgb[si % 2]
                    si += 1
                    pg, pgb = self.nps()
                    self.mm(pg[:], [(wg_[:, kc, jj * 128:(jj + 1) * 128], self.hT[:, kc, sl]) for kc in range(KC)],
                            self.hreads(tb) + [wgb_], [pgb])
                    pu, pub = self.nps()
                    self.mm(pu[:], [(wu_[:, kc, jj * 128:(jj + 1) * 128], self.hT[:, kc, sl]) for kc in range(KC)],
                            self.hreads(tb) + [wub_], [pub])
                    self.act(s_[:], pg[:], AF.Silu, [pgb], [sb_])
                    if cbrow is not None:
                        self.tt("pool", s_[:], s_[:], cbrow[:, sl], ALU.mult, [sb_, cbrowb], [sb_])
                    self.tt("dve", a[:, jj, sl], pu[:], s_[:], ALU.mult, [pub, sb_], [ab[jj][tb]])
            for c in range(KC):
                for tb in range(NTB):
                    sl = slice(tb * TB, (tb + 1) * TB)
                    pt, pb = self.nps()
                    self.mm(pt[:], [(wd_[:, jj, c * 128:(c + 1) * 128], a[:, jj, sl]) for jj in range(n)],
                            [ab[jj][tb] for jj in range(n)] + [wdb_], [pb])
                    self.tt("dve", self.xT[:, c, sl], pt[:], self.xT[:, c, sl], ALU.add, [pb, self.xTb[c][tb]],
                            [self.xTb[c][tb]])
            j0 += n

    def alloc_ffn_tmp(self):
        self.f_a = self.aalloc([128, 4, T], BF16)
        self.f_ab = [[Buf("a%d_%d" % (j, tb)) for tb in range(NTB)] for j in range(4)]
        self.f_sg = [self.aalloc([128, TB], BF16) for _ in range(2)]
        self.f_sgb = [Buf("sg0"), Buf("sg1")]

    def moe(self, l):
        S, d = self.S, self.d
        wr, wrb = self.wload(d["router_w"][l // 2].rearrange("(c p) n -> p c n", p=128),
                             lambda t: t[:, 0:64].rearrange("p (c n) -> p c n", c=KC))
        L = self.aalloc([128, NTT, NE], F32); Lb = Buf("L")
        L2 = self.aalloc([128, NTT, NE], F32)
        m1 = self.aalloc([128, NTT], F32)
        m2 = self.aalloc([128, NTT], F32)
        cmb = self.aalloc([128, NTT, NE], F32)
        pt, pb = self.nps((6, 7))
        for tt in range(NTT):
            pairs = [(self.hT[:, kc, tt * 128:(tt + 1) * 128], wr[:, kc, :]) for kc in range(KC)]
            self.mm(pt[:, tt * 8:(tt + 1) * 8], pairs, self.hreads(tt // 4) + [wrb], [pb])
        self.tt("dve", L[:], pt[:, 0:128].rearrange("p (a b) -> p a b", a=NTT),
                self.pcol(l, self.PA_RB, 8).unsqueeze(1).broadcast_to([128, NTT, NE]), ALU.add, [pb, self.parb], [Lb])
        bc = lambda t: t[:].unsqueeze(2).broadcast_to([128, NTT, NE])
        S.op("dve", lambda e: e.tensor_reduce(m1[:], L[:], AX.X, ALU.max), [Lb], [Lb])
        self.tt("dve", L2[:], L[:], bc(m1), ALU.is_equal, [Lb], [Lb])
        self.stt("dve", L2[:], L2[:], -1e30, L[:], ALU.mult, ALU.add, [Lb], [Lb])
        S.op("dve", lambda e: e.tensor_reduce(m2[:], L2[:], AX.X, ALU.max), [Lb], [Lb])
        self.tt("dve", L2[:], L[:], bc(m2), ALU.is_ge, [Lb], [Lb])
        self.tt("dve", cmb[:], L[:], bc(m1), ALU.subtract, [Lb], [Lb])
        self.act(cmb[:], cmb[:], AF.Exp, [Lb], [Lb])
        self.tt("dve", cmb[:], cmb[:], L2[:], ALU.mult, [Lb], [Lb])
        self.tt("dve", m2[:], m2[:], m1[:], ALU.subtract, [Lb], [Lb])
        self.act(m2[:], m2[:], AF.Exp, [Lb], [Lb])
        self.ts("dve", m2[:], m2[:], 1.0, None, ALU.add, None, [Lb], [Lb])
        S.op("dve", lambda e: e.reciprocal(m2[:], m2[:]), [Lb], [Lb])
        self.tt("dve", cmb[:], cmb[:], bc(m2), ALU.mult, [Lb], [Lb])
        self.tap("cmb", cmb[:], [128, NTT, NE], F32, [Lb])
        cbrow = [self.aalloc([128, T], BF16) for _ in range(2)]
        cbrowb = [Buf("cbrow0"), Buf("cbrow1")]
        dg = [self.aalloc([128, 128], F32) for _ in range(2)]; dgb = [Buf("dg0"), Buf("dg1")]

        def build_cbrow(e_):
            cr, crb = cbrow[e_ % 2], cbrowb[e_ % 2]
            for q4 in range(4):
                pt, pb = self.nps((6, 7))
                for i in range(4):
                    tt = q4 * 4 + i
                    self.ts("dve", dg[i % 2][:], self.ident_f, cmb[:, tt, e_:e_ + 1], None, ALU.mult, None,
                            [self.cfb, Lb], [dgb[i % 2]])
                    self.mm1(pt[:, i * 128:(i + 1) * 128], self.ones_f, dg[i % 2][:], [self.cfb, dgb[i % 2]], [pb])
                self.copy("act", cr[:, q4 * TB:(q4 + 1) * TB], pt[:], [pb], [crb])

        build_cbrow(0)
        for e_ in range(NE):
            if e_ + 1 < NE:
                build_cbrow(e_ + 1)
            self.ffn(l, e_, cbrow[e_ % 2], cbrowb[e_ % 2])

    def ple(self, l):
        S, d = self.S, self.d
        pT = self.aalloc([128, 2, T], BF16)
        pTb = [Buf("pT%d" % tb) for tb in range(NTB)]
        stg = [self.aalloc([128, PLE], F32) for _ in range(2)]
        stgb = [Buf("pst0"), Buf("pst1")]
        pgt = [self.aalloc([128, TB], F32) for _ in range(2)]
        pgb_ = [Buf("pg0"), Buf("pg1")]
        for tt in range(NTT):
            s, sb_ = stg[tt % 2], stgb[tt % 2]
            S.dma("sp", s, d["p"][l, tt * 128:(tt + 1) * 128, :], writes=[sb_])
            pt, pb = self.nps()
            for i in range(2):
                self.transpose(pt[:, i * 128:(i + 1) * 128], s[:, i * 128:(i + 1) * 128], self.ident_f, [sb_, self.cfb], [pb])
            self.copy("act", pT[:, :, tt * 128:(tt + 1) * 128], pt[:, 0:256].rearrange("p (a b) -> p a b", a=2), [pb],
                      [pTb[tt // 4]])
        gi = 0
        for cg in range(2):
            wg, wgb = self.wload(d["ple_w_gate"][l][:, cg * 512:(cg + 1) * 512].rearrange("(c p) n -> p c n", p=128),
                                 lambda t: t[:].rearrange("p (c n) -> p c n", c=KC))
            wp, wpb = self.wload(d["ple_w_proj"][l][:, cg * 512:(cg + 1) * 512].rearrange("(c p) n -> p c n", p=128),
                                 lambda t: t[:, 0:1024].rearrange("p (c n) -> p c n", c=2))
            for m in range(4):
                c = cg * 4 + m
                for tb in range(NTB):
                    sl = slice(tb * TB, (tb + 1) * TB)
                    g_, gb_ = pgt[gi % 2], pgb_[gi % 2]
                    gi += 1
                    pt, pb = self.nps()
                    self.mm(pt[:], [(wg[:, kc, m * 128:(m + 1) * 128], self.hT[:, kc, sl]) for kc in range(KC)],
                            self.hreads(tb) + [wgb], [pb])
                    self.act(g_[:], pt[:], AF.Sigmoid, [pb], [gb_])
                    pt2, pb2 = self.nps()
                    self.mm(pt2[:], [(wp[:, kc, m * 128:(m + 1) * 128], pT[:, kc, sl]) for kc in range(2)],
                            [pTb[tb], wpb], [pb2])
                    self.tt("dve", g_[:], pt2[:], g_[:], ALU.mult, [pb2, gb_], [gb_])
                    self.tt("dve", self.xT[:, c, sl], self.xT[:, c, sl], g_[:], ALU.add, [self.xTb[c][tb], gb_],
                            [self.xTb[c][tb]])


_INPUT_NAMES = ["attn_norm", "w_in", "m_conv_w", "m_conv_b", "m_gate_bias", "m_head_norm", "a_q_norm", "a_k_norm",
                "a_lambda", "a_head_norm", "pool_w", "pool_scale", "w_branch", "w_out", "ffn_norm", "dense_w_gu",
                "dense_w_down", "router_w", "router_b", "moe_w_gu", "moe_w_down", "ple_norm", "ple_w_gate", "ple_w_proj"]


def make_in_maps(inputs, cores):
    consts = make_consts()
    maps = []
    for b in cores:
        m = {"x": np.ascontiguousarray(inputs["x"][b], dtype=np.float32),
             "p": np.ascontiguousarray(inputs["p"][:, b], dtype=np.float32),
             "consts": consts}
        for k in _INPUT_NAMES:
            m[k] = np.ascontiguousarray(inputs[k], dtype=np.float32)
        maps.append(m)
    return maps


def kernel(**inputs):
    prog = Prog()
    nc = prog.build()
    in_maps = make_in_maps(inputs, list(range(8)))
    res = run_bass_kernel_spmd(nc, in_maps, core_ids=list(range(8)))
    out = np.stack([np.asarray(r["out"], dtype=np.float32) for r in res.results], axis=0)
    return out
```
